# Optimizing a Trainium2 kernel written in Bass

```python
import math
import jax, jax.numpy as jnp
from jax import lax
import numpy as np

D_MODEL = 2048
BATCH = 4
SEQ = 4096
DEPTH = 2

GRID_W = 64
ROPE_THETA = 10000.0
N_MIXERS = 2
ATTN_HEADS = 16
ATTN_KV_HEADS = 4
ATTN_HEAD_DIM = D_MODEL // ATTN_HEADS
ATTN_GROUP = ATTN_HEADS // ATTN_KV_HEADS
Q_BLOCK = 128
QK_NORM_EPS = 1e-6
RET_HEADS = 8
RET_QK_DIM = D_MODEL // RET_HEADS
RET_V_DIM = 2 * RET_QK_DIM
RET_CHUNK = 128
RET_GN_EPS = 1e-5
N_EXPERTS = 32
TOP_K = 4
D_EXPERT = D_MODEL
SWIGLU_LIMIT = 7.0
SWIGLU_ALPHA = 1.702
MOE_BLOCK = 256
LN_EPS = 1e-5
DEEPNORM_ALPHA = (2 * DEPTH) ** 0.25
DEEPNORM_BETA = (8 * DEPTH) ** -0.25
N_ATTN_LAYERS = (DEPTH + 1) // 2
N_RET_LAYERS = DEPTH // 2

kernel_name = "hybrid_gqa_retention_moe_encoder"


def layer_norm(x, g, b):
    xf = x.astype(jnp.float32)
    mu = jnp.mean(xf, axis=-1, keepdims=True)
    var = jnp.mean(jnp.square(xf - mu), axis=-1, keepdims=True)
    return ((xf - mu) * lax.rsqrt(var + LN_EPS) * g.astype(jnp.float32) + b.astype(jnp.float32)).astype(x.dtype)


def rms_norm_heads(x, g):
    xf = x.astype(jnp.float32)
    return xf * lax.rsqrt(jnp.mean(jnp.square(xf), axis=-1, keepdims=True) + QK_NORM_EPS) * g.astype(jnp.float32)


def axial_rope_tables(seq, dim):
    rows = seq // GRID_W
    row_idx = jnp.repeat(jnp.arange(rows, dtype=jnp.float32), GRID_W)
    col_idx = jnp.tile(jnp.arange(GRID_W, dtype=jnp.float32), rows)
    quarter = dim // 4
    inv_freq = ROPE_THETA ** (-jnp.arange(quarter, dtype=jnp.float32) / quarter)
    ang_r = row_idx[:, None] * inv_freq[None, :]
    ang_c = col_idx[:, None] * inv_freq[None, :]
    ang = jnp.concatenate([ang_r, ang_r, ang_c, ang_c], axis=-1)
    return jnp.cos(ang), jnp.sin(ang)


def _rotate_half(x):
    h = x.shape[-1] // 2
    return jnp.concatenate([-x[..., h:], x[..., :h]], axis=-1)


def apply_axial_rope(x, cos, sin):
    half = x.shape[-1] // 2
    rot = jnp.concatenate([_rotate_half(x[..., :half]), _rotate_half(x[..., half:])], axis=-1)
    return x * cos[None, :, None, :] + rot * sin[None, :, None, :]


def attention_mixer(x, w_in, q_gain, k_gain, w_out, cos, sin):
    bsz, seq, _ = x.shape
    h = x @ w_in
    nq = ATTN_HEADS * ATTN_HEAD_DIM
    nkv = ATTN_KV_HEADS * ATTN_HEAD_DIM
    q, k, v = jnp.split(h, [nq, nq + nkv], axis=-1)
    q = q.reshape(bsz, seq, ATTN_HEADS, ATTN_HEAD_DIM)
    k = k.reshape(bsz, seq, ATTN_KV_HEADS, ATTN_HEAD_DIM)
    v = v.reshape(bsz, seq, ATTN_KV_HEADS, ATTN_HEAD_DIM)
    q = (apply_axial_rope(rms_norm_heads(q, q_gain), cos, sin) * (ATTN_HEAD_DIM ** -0.5)).astype(x.dtype)
    k = apply_axial_rope(rms_norm_heads(k, k_gain), cos, sin).astype(x.dtype)
    n_blocks = seq // Q_BLOCK
    qb = q.reshape(bsz, n_blocks, Q_BLOCK, ATTN_KV_HEADS, ATTN_GROUP, ATTN_HEAD_DIM).transpose(1, 0, 3, 4, 2, 5)
    kt = k.transpose(0, 2, 1, 3)
    vt = v.transpose(0, 2, 1, 3)

    def one_block(q_blk):
        s = jnp.einsum('bhgqd,bhkd->bhgqk', q_blk, kt).astype(jnp.float32)
        p = jax.nn.softmax(s, axis=-1).astype(vt.dtype)
        return jnp.einsum('bhgqk,bhkd->bhgqd', p, vt)

    o = lax.map(one_block, qb)
    o = o.transpose(1, 0, 4, 2, 3, 5).reshape(bsz, seq, nq)
    return o @ w_out


def retention_chunkwise(q, k, v, log_gamma):
    bsz, seq, nh, dk = q.shape
    dv = v.shape[-1]
    c = RET_CHUNK
    nc = seq // c
    to_chunks = lambda t: t.reshape(bsz, nc, c, nh, t.shape[-1]).transpose(1, 0, 3, 2, 4)
    qc, kc, vc = to_chunks(q), to_chunks(k), to_chunks(v)
    pos = jnp.arange(c, dtype=jnp.float32)
    dist = pos[:, None] - pos[None, :]
    lg = log_gamma.astype(jnp.float32)
    intra = jnp.where(dist[None] >= 0, jnp.exp(lg[:, None, None] * jnp.maximum(dist, 0.0)[None]), 0.0)
    q_decay = jnp.exp(lg[:, None] * (pos[None, :] + 1.0))
    k_decay = jnp.exp(lg[:, None] * (c - 1.0 - pos[None, :]))
    chunk_decay = jnp.exp(lg * c)

    def step(state, chunk):
        qi, ki, vi = chunk
        scores = jnp.einsum('bhqd,bhkd->bhqk', qi, ki) * intra[None]
        o = (jnp.einsum('bhqk,bhkv->bhqv', scores, vi)
             + jnp.einsum('bhqd,bhdv->bhqv', qi * q_decay[None, :, :, None], state))
        state = (state * chunk_decay[None, :, None, None]
                 + jnp.einsum('bhkd,bhkv->bhdv', ki * k_decay[None, :, :, None], vi))
        return state, o

    state0 = jnp.zeros((bsz, nh, dk, dv), jnp.float32)
    _, o = lax.scan(step, state0, (qc, kc, vc))
    return o.transpose(1, 0, 3, 2, 4).reshape(bsz, seq, nh, dv)


def retention_mixer(x, w_in, decay_fwd, decay_bwd, w_out, cos, sin):
    bsz, seq, _ = x.shape
    nqk = RET_HEADS * RET_QK_DIM
    nv = RET_HEADS * RET_V_DIM
    h = x @ w_in
    q, k, v, g = jnp.split(h, [nqk, 2 * nqk, 2 * nqk + nv], axis=-1)
    q = apply_axial_rope(q.astype(jnp.float32).reshape(bsz, seq, RET_HEADS, RET_QK_DIM), cos, sin)
    k = apply_axial_rope(k.astype(jnp.float32).reshape(bsz, seq, RET_HEADS, RET_QK_DIM), cos, sin) * (RET_QK_DIM ** -0.5)
    v = v.astype(jnp.float32).reshape(bsz, seq, RET_HEADS, RET_V_DIM)
    lg_f = jnp.log1p(-jnp.exp(decay_fwd.astype(jnp.float32)))
    lg_b = jnp.log1p(-jnp.exp(decay_bwd.astype(jnp.float32)))
    o_f = retention_chunkwise(q, k, v, lg_f)
    o_b = jnp.flip(retention_chunkwise(jnp.flip(q, 1), jnp.flip(k, 1), jnp.flip(v, 1), lg_b), 1)
    o = o_f + o_b
    mu = jnp.mean(o, axis=-1, keepdims=True)
    var = jnp.mean(jnp.square(o - mu), axis=-1, keepdims=True)
    o = ((o - mu) * lax.rsqrt(var + RET_GN_EPS)).reshape(bsz, seq, nv).astype(x.dtype)
    return (jax.nn.silu(g) * o) @ w_out


def moe_ffn(x2, router_w, router_b, w_gate_up, b_gate_up, w_down, b_down):
    t = x2.shape[0]
    logits = (x2 @ router_w).astype(jnp.float32) + router_b.astype(jnp.float32)
    top_vals, top_idx = lax.top_k(logits, TOP_K)
    gates = jax.nn.softmax(top_vals, axis=-1).astype(x2.dtype)
    n_assign = t * TOP_K
    n_blocks = -(-n_assign // MOE_BLOCK) + N_EXPERTS
    flat_e = top_idx.reshape(-1).astype(jnp.int32)
    flat_tok = jnp.repeat(jnp.arange(t, dtype=jnp.int32), TOP_K)
    flat_gate = gates.reshape(-1)
    order = jnp.argsort(flat_e)
    sorted_e = flat_e[order]
    counts = jnp.bincount(flat_e, length=N_EXPERTS).astype(jnp.int32)
    start = jnp.cumsum(counts) - counts
    padded = (counts + MOE_BLOCK - 1) // MOE_BLOCK * MOE_BLOCK
    pad_end = jnp.cumsum(padded)
    pad_start = pad_end - padded
    dest = pad_start[sorted_e] + (jnp.arange(n_assign, dtype=jnp.int32) - start[sorted_e])
    buf_tok = jnp.zeros((n_blocks * MOE_BLOCK,), jnp.int32).at[dest].set(flat_tok[order])
    buf_gate = jnp.zeros((n_blocks * MOE_BLOCK,), x2.dtype).at[dest].set(flat_gate[order])
    block_start = jnp.arange(n_blocks, dtype=jnp.int32) * MOE_BLOCK
    block_e = jnp.minimum(jnp.searchsorted(pad_end, block_start, side='right'), N_EXPERTS - 1).astype(jnp.int32)

    def expert_block(args):
        tok, e, gate = args
        xb = x2[tok]
        gu = xb @ w_gate_up[e] + b_gate_up[e]
        hg = jnp.minimum(gu[:, 0::2], SWIGLU_LIMIT)
        hu = jnp.clip(gu[:, 1::2], -SWIGLU_LIMIT, SWIGLU_LIMIT)
        act = (hu + 1.0) * (hg * jax.nn.sigmoid(SWIGLU_ALPHA * hg))
        return (act @ w_down[e] + b_down[e]) * gate[:, None]

    y = lax.map(expert_block, (buf_tok.reshape(n_blocks, MOE_BLOCK), block_e,
                               buf_gate.reshape(n_blocks, MOE_BLOCK)))
    y = y.reshape(n_blocks * MOE_BLOCK, -1)
    return jax.ops.segment_sum(y, buf_tok, num_segments=t).astype(x2.dtype)


def setup_inputs(seed: int = 0) -> dict:
    key = jax.random.key(seed)
    ks = jax.random.split(key, 20)
    f32 = jnp.float32
    d = D_MODEL
    nq = ATTN_HEADS * ATTN_HEAD_DIM
    nkv = ATTN_KV_HEADS * ATTN_HEAD_DIM
    nqk = RET_HEADS * RET_QK_DIM
    nv = RET_HEADS * RET_V_DIM
    x = jax.random.normal(ks[0], (BATCH, SEQ, d), f32)
    attn_w_in = jax.random.normal(ks[1], (N_ATTN_LAYERS, d, nq + 2 * nkv), f32) * d ** -0.5
    attn_w_in = attn_w_in.at[..., nq + nkv:].multiply(DEEPNORM_BETA)
    attn_q_gain = 1.0 + 0.02 * jax.random.normal(ks[2], (N_ATTN_LAYERS, ATTN_HEAD_DIM), f32)
    attn_k_gain = 1.0 + 0.02 * jax.random.normal(ks[3], (N_ATTN_LAYERS, ATTN_HEAD_DIM), f32)
    attn_w_out = jax.random.normal(ks[4], (N_ATTN_LAYERS, nq, d), f32) * (nq ** -0.5) * DEEPNORM_BETA
    ret_w_in = jax.random.normal(ks[5], (N_RET_LAYERS, d, 2 * nqk + 2 * nv), f32) * d ** -0.5
    ret_w_in = ret_w_in.at[..., 2 * nqk:2 * nqk + nv].multiply(DEEPNORM_BETA)
    base_decay = jnp.log(2.0 ** (-5.0 - jnp.arange(RET_HEADS, dtype=f32)))
    ret_decay_fwd = base_decay[None] + 0.05 * jax.random.normal(ks[6], (N_RET_LAYERS, RET_HEADS), f32)
    ret_decay_bwd = base_decay[None] + 0.05 * jax.random.normal(ks[7], (N_RET_LAYERS, RET_HEADS), f32)
    ret_w_out = jax.random.normal(ks[8], (N_RET_LAYERS, nv, d), f32) * (nv ** -0.5) * DEEPNORM_BETA
    ln_mix_g = 1.0 + 0.02 * jax.random.normal(ks[9], (DEPTH, d), f32)
    ln_mix_b = 0.02 * jax.random.normal(ks[10], (DEPTH, d), f32)
    router_w = jax.random.normal(ks[11], (DEPTH, d, N_EXPERTS), f32) * d ** -0.5
    router_b = 0.01 * jax.random.normal(ks[12], (DEPTH, N_EXPERTS), f32)
    expert_w_gate_up = jax.random.normal(ks[13], (DEPTH, N_EXPERTS, d, 2 * D_EXPERT), f32) * d ** -0.5
    expert_b_gate_up = 0.01 * jax.random.normal(ks[14], (DEPTH, N_EXPERTS, 2 * D_EXPERT), f32)
    expert_w_down = jax.random.normal(ks[15], (DEPTH, N_EXPERTS, D_EXPERT, d), f32) * (D_EXPERT ** -0.5) * DEEPNORM_BETA
    expert_b_down = 0.01 * jax.random.normal(ks[16], (DEPTH, N_EXPERTS, d), f32)
    ln_ffn_g = 1.0 + 0.02 * jax.random.normal(ks[17], (DEPTH, d), f32)
    ln_ffn_b = 0.02 * jax.random.normal(ks[18], (DEPTH, d), f32)
    return {"x": x, "attn_w_in": attn_w_in, "attn_q_gain": attn_q_gain, "attn_k_gain": attn_k_gain,
            "attn_w_out": attn_w_out, "ret_w_in": ret_w_in, "ret_decay_fwd": ret_decay_fwd,
            "ret_decay_bwd": ret_decay_bwd, "ret_w_out": ret_w_out, "ln_mix_g": ln_mix_g, "ln_mix_b": ln_mix_b,
            "router_w": router_w, "router_b": router_b, "expert_w_gate_up": expert_w_gate_up,
            "expert_b_gate_up": expert_b_gate_up, "expert_w_down": expert_w_down, "expert_b_down": expert_b_down,
            "ln_ffn_g": ln_ffn_g, "ln_ffn_b": ln_ffn_b}


def reference(x, attn_w_in, attn_q_gain, attn_k_gain, attn_w_out, ret_w_in, ret_decay_fwd, ret_decay_bwd,
              ret_w_out, ln_mix_g, ln_mix_b, router_w, router_b, expert_w_gate_up, expert_b_gate_up,
              expert_w_down, expert_b_down, ln_ffn_g, ln_ffn_b):
    bsz, seq, d = x.shape
    cos_a, sin_a = axial_rope_tables(seq, ATTN_HEAD_DIM)
    cos_r, sin_r = axial_rope_tables(seq, RET_QK_DIM)
    for i in range(DEPTH):
        j = i // N_MIXERS
        if i % N_MIXERS == 0:
            mix = attention_mixer(x, attn_w_in[j], attn_q_gain[j], attn_k_gain[j], attn_w_out[j], cos_a, sin_a)
        else:
            mix = retention_mixer(x, ret_w_in[j], ret_decay_fwd[j], ret_decay_bwd[j], ret_w_out[j], cos_r, sin_r)
        x = layer_norm(DEEPNORM_ALPHA * x + mix, ln_mix_g[i], ln_mix_b[i])
        ffn = moe_ffn(x.reshape(bsz * seq, d), router_w[i], router_b[i], expert_w_gate_up[i],
                      expert_b_gate_up[i], expert_w_down[i], expert_b_down[i]).reshape(bsz, seq, d)
        x = layer_norm(DEEPNORM_ALPHA * x + ffn, ln_ffn_g[i], ln_ffn_b[i])
    return x
```

```python
import contextlib
import math
import numpy as np
import ml_dtypes
import concourse.bass as bass
import concourse.mybir as mybir
from concourse.bass_utils import run_bass_kernel_spmd

F32 = mybir.dt.float32
BF16 = mybir.dt.bfloat16
AF = mybir.ActivationFunctionType
ALU = mybir.AluOpType
AX = mybir.AxisListType
NPBF = ml_dtypes.bfloat16

STREAMS = ("tensor", "vector", "scalar", "gpsimd", "sync")
COUNTERS = ("tensor", "vector", "scalar", "gpsimd", "d_sync", "d_scalar", "d_gpsimd")

D = 2048
ALPHA = 4 ** 0.25
LN_EPS = 1e-5


class Prog:
    def __init__(self, nc):
        self.nc = nc
        self.ops = {s: [] for s in STREAMS}
        self.cnt = {c: 0 for c in COUNTERS}
        self.seen = {s: {c: 0 for c in COUNTERS} for s in STREAMS}
        self.lastw = {}
        self.readers = {}
        self.pending = {s: {} for s in STREAMS}

    def barrier(self):
        for s in STREAMS:
            for c, v in self.cnt.items():
                if c == "d_gpsimd":
                    continue
                if v > self.pending[s].get(c, 0):
                    self.pending[s][c] = v

    def _add(self, stream, counter, inc, fn, reads, writes):
        need = dict(self.pending[stream])
        self.pending[stream] = {}
        for k in reads:
            w = self.lastw.get(k)
            if w is not None:
                need[w[0]] = max(need.get(w[0], 0), w[1])
        for k in writes:
            w = self.lastw.get(k)
            if w is not None:
                need[w[0]] = max(need.get(w[0], 0), w[1])
            for (c, v) in self.readers.get(k, ()):
                need[c] = max(need.get(c, 0), v)
        waits = []
        for c, v in need.items():
            if c == "tensor" and counter == "tensor":
                continue
            if self.seen[stream][c] < v:
                waits.append((c, v))
                self.seen[stream][c] = v
        self.cnt[counter] += inc
        myv = self.cnt[counter]
        self.ops[stream].append((waits, fn, counter, inc))
        for k in reads:
            self.readers.setdefault(k, []).append((counter, myv))
        for k in writes:
            self.lastw[k] = (counter, myv)
            self.readers[k] = []

    def op(self, eng, fn, reads=(), writes=()):
        self._add(eng, eng, 1, fn, reads, writes)

    def dma(self, q, fn, reads=(), writes=()):
        self._add(q, "d_" + q, 16, fn, reads, writes)

    def emit(self):
        nc = self.nc
        with contextlib.ExitStack() as st:
            sems = {c: st.enter_context(nc.semaphore("s_" + c)) for c in COUNTERS}
            block = st.enter_context(nc.Block())
            final_cnt = dict(self.cnt)

            def make(stream):
                def body(eng):
                    for (waits, fn, counter, inc) in self.ops[stream]:
                        for (c, v) in waits:
                            eng.wait_ge(sems[c], v)
                        fn(eng).then_inc(sems[counter], inc)
                    if stream == "sync":
                        for c in COUNTERS:
                            if final_cnt[c] > 0:
                                eng.wait_ge(sems[c], final_cnt[c])
                return body

            block.tensor(make("tensor"))
            block.vector(make("vector"))
            block.scalar(make("scalar"))
            block.gpsimd(make("gpsimd"))
            block.sync(make("sync"))


class Ctx:
    def __init__(self):
        self.nc = bass.Bass("TRN2", target_bir_lowering=False)
        self.P = Prog(self.nc)
        self.st = contextlib.ExitStack()
        self.n = 0

    def din(self, name, shape, dt=F32):
        return self.nc.dram_tensor(name, list(shape), dt, kind="ExternalInput").ap()

    def dout(self, name, shape, dt=F32):
        return self.nc.dram_tensor(name, list(shape), dt, kind="ExternalOutput").ap()

    def dscr(self, name, shape, dt=F32):
        return self.nc.dram_tensor(name, list(shape), dt).ap()

    pfx = ""

    def sb(self, name, shape, dt=F32, st=None):
        return (st or self.st).enter_context(self.nc.sbuf_tensor(self.pfx + name, list(shape), dt))

    def ps(self, name, shape=(128, 512), dt=F32, st=None):
        return (st or self.st).enter_context(self.nc.psum_tensor(self.pfx + name, list(shape), dt))

    def finish(self):
        self.P.emit()
        self.st.close()
        return self.nc


def rope_tile(C, T, src_ps, src_key, out_ap, out_key, cos_ap, sin_ap, tab_keys, gain_ap, rms):
    P = C.P
    q0, sq, rstd, qn, t1, t2 = T.get("q0"), T.get("sq"), T.get("rstd"), T["qn"], T["t1"], T["t2"]
    psa, psb = T["psa"], T["psb"]
    if rms:
        P.op("scalar", lambda q: q.activation(out=q0[:], in_=src_ps[:], func=AF.Copy), reads=[src_key], writes=["q0"])
        P.op("scalar", lambda q: q.activation(out=sq[:], in_=src_ps[:], func=AF.Square), reads=[src_key], writes=["sq"])
        P.op("tensor", lambda q: q.matmul(psa[:], T["ones"][:], sq[:], start=True, stop=True), reads=["sq", "consts"], writes=["psa"])
        P.op("scalar", lambda q: q.activation(out=rstd[:], in_=psa[:], func=AF.Sqrt, scale=1.0 / 128, bias=1e-6), reads=["psa"], writes=["rstd"])
        P.op("vector", lambda q: q.reciprocal(out=rstd[:], in_=rstd[:]), reads=["rstd"], writes=["rstd"])
        P.op("vector", lambda q: q.scalar_tensor_tensor(out=qn[:], in0=q0[:], scalar=gain_ap, in1=rstd[:], op0=ALU.mult, op1=ALU.mult),
             reads=["q0", "rstd", "consts"], writes=["qn"])
    else:
        P.op("scalar", lambda q: q.activation(out=qn[:], in_=src_ps[:], func=AF.Copy, scale=gain_ap), reads=[src_key, "consts"], writes=["qn"])
    P.op("tensor", lambda q: q.matmul(psb[:], T["rot"][:], qn[:], start=True, stop=True), reads=["qn", "consts"], writes=["psb"])
    P.op("gpsimd", lambda q: q.tensor_tensor(out=t1[:], in0=qn[:], in1=cos_ap, op=ALU.mult), reads=["qn"] + tab_keys, writes=["t1"])
    P.op("vector", lambda q: q.tensor_tensor(out=t2[:], in0=psb[:], in1=sin_ap, op=ALU.mult), reads=["psb"] + tab_keys, writes=["t2"])
    P.op("vector", lambda q: q.tensor_tensor(out=out_ap, in0=t1[:], in1=t2[:], op=ALU.add), reads=["t1", "t2"], writes=[out_key])


def rope_temps(C, st):
    T = {}
    for n in ("q0", "sq", "rstd", "qn", "t1", "t2"):
        T[n] = C.sb(n, [128, 512], F32, st=st)
    T["psa"] = C.ps("psa", st=st); T["psb"] = C.ps("psb", st=st)
    return T


def rope_tables(seq, dim):
    rows = seq // 64
    row_idx = np.repeat(np.arange(rows, dtype=np.float32), 64)
    col_idx = np.tile(np.arange(64, dtype=np.float32), rows)
    quarter = dim // 4
    inv_freq = (10000.0 ** (-np.arange(quarter, dtype=np.float32) / quarter)).astype(np.float32)
    ang_r = row_idx[:, None] * inv_freq[None, :]
    ang_c = col_idx[:, None] * inv_freq[None, :]
    ang = np.concatenate([ang_r, ang_r, ang_c, ang_c], axis=-1).astype(np.float32)
    return np.cos(ang).astype(np.float32), np.sin(ang).astype(np.float32)


def rot_lhsT(block):
    R = np.zeros((128, 128), np.float32)
    h = block // 2
    for d in range(128):
        b0 = (d // block) * block; o = d % block
        if o < h:
            R[b0 + o + h, d] = -1.0
        else:
            R[b0 + o - h, d] = 1.0
    return R


NT = 4096
NEXP = 32


def emit_ln(C, pfx, xold, part, g, b, rwT, rb, identb, xnew, xT_out, gates):
    P = C.P; C.pfx = pfx
    P.barrier()
    with contextlib.ExitStack() as S:
        xn = C.sb("xn", [128, 8, D], st=S); pt = C.sb("pt", [128, D], st=S)
        junk = C.sb("junk", [128, D], st=S); jbf = C.sb("jbf", [128, D], BF16, st=S); abf = C.sb("abf", [128, D], BF16, st=S)
        tT = C.sb("tT", [128, 16, 128], BF16, st=S); idb = C.sb("idb", [128, 128], BF16, st=S)
        gb = C.sb("gb", [128, D], st=S); bb = C.sb("bb", [128, D], st=S); rwb = [C.sb("rwb%d" % i, [128, D], st=S) for i in range(2)]
        rbb = C.sb("rbb", [128, 32], st=S); st = C.sb("st", [128, 8], st=S); lg = C.sb("lg", [128, 8, 32], st=S)
        lgt = C.sb("lgt", [128, 32], st=S); m8 = C.sb("m8", [128, 8], st=S); mask = C.sb("mask", [128, 32], st=S); ex = C.sb("ex", [128, 32], st=S)
        sm = C.sb("sm", [128, 4], st=S); gt = C.sb("gt", [128, 32], st=S)
        pst = [C.ps("pst%d" % i, [128, 8, 128], BF16, st=S) for i in range(2)]
        P.dma("sync", lambda q: q.dma_start(out=gb[:], in_=g.partition_broadcast(128)), writes=["gb"])
        P.dma("sync", lambda q: q.dma_start(out=bb[:], in_=b.partition_broadcast(128)), writes=["bb"])
        P.dma("sync", lambda q: q.dma_start(out=idb[:], in_=identb), writes=["idb"])
        if gates is not None:
            P.dma("sync", lambda q: q.dma_start(out=rbb[:], in_=rb.partition_broadcast(128)), writes=["rbb"])
        for grp in range(NT // 1024):
            if gates is not None:
                P.op("vector", lambda q: q.memset(lg[:], 0.0), writes=["lg"])
            for tl in range(8):
                r0 = (grp * 8 + tl) * 128
                A = xn[:, tl, :]; ak = "xn%d" % tl
                P.dma("sync", lambda q, A=A, r0=r0: q.dma_start(out=A, in_=xold[r0:r0 + 128, :]), reads=["dram_x"], writes=[ak])
                P.dma("sync", lambda q, r0=r0: q.dma_start(out=pt[:], in_=part[r0:r0 + 128, :]), reads=["dram_part"], writes=["pt"])
                P.op("vector", lambda q, A=A: q.scalar_tensor_tensor(out=A, in0=A, scalar=ALPHA, in1=pt[:], op0=ALU.mult, op1=ALU.add),
                     reads=[ak, "pt"], writes=[ak])
                P.op("vector", lambda q: q.memset(st[:], 0.0), writes=["st"])
                P.op("scalar", lambda q, A=A: q.activation(out=jbf[:], in_=A, func=AF.Copy, accum_out=st[:, 0:1]), reads=[ak, "st"], writes=["jbf", "st"])
                P.op("scalar", lambda q, A=A: q.activation(out=jbf[:], in_=A, func=AF.Square, accum_out=st[:, 1:2]), reads=[ak, "st"], writes=["jbf", "st"])
                P.op("vector", lambda q: q.tensor_scalar(out=st[:, 2:3], in0=st[:, 0:1], scalar1=1.0 / D, scalar2=None, op0=ALU.mult), reads=["st"], writes=["st"])
                P.op("vector", lambda q: q.tensor_tensor(out=st[:, 3:4], in0=st[:, 2:3], in1=st[:, 2:3], op=ALU.mult), reads=["st"], writes=["st"])
                P.op("vector", lambda q: q.scalar_tensor_tensor(out=st[:, 4:5], in0=st[:, 1:2], scalar=1.0 / D, in1=st[:, 3:4], op0=ALU.mult, op1=ALU.subtract),
                     reads=["st"], writes=["st"])
                P.op("scalar", lambda q: q.activation(out=st[:, 5:6], in_=st[:, 4:5], func=AF.Sqrt, bias=LN_EPS, scale=1.0), reads=["st"], writes=["st"])
                P.op("vector", lambda q: q.reciprocal(out=st[:, 6:7], in_=st[:, 5:6]), reads=["st"], writes=["st"])
                P.op("vector", lambda q, A=A: q.tensor_scalar(out=A, in0=A, scalar1=st[:, 2:3], scalar2=st[:, 6:7], op0=ALU.subtract, op1=ALU.mult),
                     reads=[ak, "st"], writes=[ak])
                P.op("vector", lambda q, A=A: q.tensor_tensor(out=A, in0=A, in1=gb[:], op=ALU.mult), reads=[ak, "gb"], writes=[ak])
                P.op("vector", lambda q, A=A: q.tensor_tensor(out=A, in0=A, in1=bb[:], op=ALU.add), reads=[ak, "bb"], writes=[ak])
                P.dma("sync", lambda q, A=A, r0=r0: q.dma_start(out=xnew[r0:r0 + 128, :], in_=A), reads=[ak], writes=["dram_xnew"])
                if xT_out is not None:
                    P.op("scalar", lambda q, A=A: q.activation(out=abf[:], in_=A, func=AF.Copy), reads=[ak], writes=["abf"])
                    for j in range(16):
                        P.op("tensor", lambda q, j=j: q.transpose(pst[j // 8][:, j % 8, :], abf[:, j * 128:(j + 1) * 128], idb[:]),
                             reads=["abf", "idb"], writes=["pst%d" % (j // 8)])
                    for i2 in range(2):
                        P.op("vector", lambda q, i2=i2: q.tensor_copy(tT[:, i2 * 8:(i2 + 1) * 8, :], pst[i2][:]), reads=["pst%d" % i2], writes=["tT"])
                    P.dma("sync", lambda q, r0=r0: q.dma_start(out=xT_out[:, r0:r0 + 128].rearrange("(kc p) t -> p kc t", p=128), in_=tT[:]),
                          reads=["tT"], writes=["dram_xT"])
            if gates is None:
                continue
            for e in range(32):
                W = rwb[e % 2]; wk = "rwb%d" % (e % 2)
                P.dma("sync", lambda q, W=W, e=e: q.dma_start(out=W[:], in_=rwT[e:e + 1, :].partition_broadcast(128)), writes=[wk])
                for tl in range(8):
                    P.op("vector", lambda q, W=W, tl=tl, e=e: q.scalar_tensor_tensor(out=junk[:], in0=xn[:, tl, :], scalar=1.0, in1=W[:], op0=ALU.mult,
                                                                                      op1=ALU.mult, accum_out=lg[:, tl, e:e + 1]),
                         reads=["xn%d" % tl, wk, "lg"], writes=["junk", "lg"])
            for tl in range(8):
                r0 = (grp * 8 + tl) * 128
                P.op("vector", lambda q, tl=tl: q.tensor_tensor(out=lgt[:], in0=lg[:, tl, :], in1=rbb[:], op=ALU.add), reads=["lg", "rbb"], writes=["lgt"])
                P.op("vector", lambda q: q.max(out=m8[:], in_=lgt[:]), reads=["lgt"], writes=["m8"])
                P.op("vector", lambda q: q.tensor_scalar(out=mask[:], in0=lgt[:], scalar1=m8[:, 3:4], scalar2=None, op0=ALU.is_ge), reads=["lgt", "m8"], writes=["mask"])
                P.op("vector", lambda q: q.tensor_scalar(out=sm[:, 0:1], in0=m8[:, 0:1], scalar1=-1.0, scalar2=None, op0=ALU.mult), reads=["m8"], writes=["sm"])
                P.op("scalar", lambda q: q.activation(out=ex[:], in_=lgt[:], func=AF.Exp, bias=sm[:, 0:1], scale=1.0), reads=["lgt", "sm"], writes=["ex"])
                P.op("vector", lambda q: q.tensor_tensor(out=ex[:], in0=ex[:], in1=mask[:], op=ALU.mult), reads=["ex", "mask"], writes=["ex"])
                P.op("vector", lambda q: q.reduce_sum(out=sm[:, 1:2], in_=ex[:], axis=AX.X), reads=["ex", "sm"], writes=["sm"])
                P.op("vector", lambda q: q.reciprocal(out=sm[:, 2:3], in_=sm[:, 1:2]), reads=["sm"], writes=["sm"])
                P.op("vector", lambda q: q.tensor_scalar(out=gt[:], in0=ex[:], scalar1=sm[:, 2:3], scalar2=None, op0=ALU.mult), reads=["ex", "sm"], writes=["gt"])
                P.dma("sync", lambda q, r0=r0: q.dma_start(out=gates[r0:r0 + 128, :], in_=gt[:]), reads=["gt"], writes=["dram_gates"])
    P.barrier()


MOE_BLK = 512


def emit_moe_convert(C, wgu, wd, wgu_s, wd_s):
    P = C.P
    for e in range(NEXP):
        for j in range(16):
            i = e * 16 + j
            P.dma("gpsimd", lambda q, i=i, e=e, j=j: q.dma_start(out=wgu_s[e][j * 128:(j + 1) * 128, :], in_=wgu[i * 128:(i + 1) * 128, :]), writes=[wgu_s[e].name])
            P.dma("gpsimd", lambda q, i=i, e=e, j=j: q.dma_start(out=wd_s[e][j * 128:(j + 1) * 128, :], in_=wd[i * 128:(i + 1) * 128, :]), writes=[wd_s[e].name])


def emit_moe(C, pfx, xT, gl, wgu_s, wd_s, bgu, bd, y):
    P = C.P; C.pfx = pfx
    P.barrier()
    with contextlib.ExitStack() as S:
        xb = C.sb("xb", [128, 16, MOE_BLK], BF16, st=S); act = C.sb("act", [128, 16, MOE_BLK], BF16, st=S)
        yacc = C.sb("yacc", [128, 4, D], st=S); wdt = C.sb("wdt", [128, 16, D], BF16, st=S)
        wt = [C.sb("wt%d" % i, [128, 16, 256], BF16, st=S) for i in range(2)]
        bgt = C.sb("bgt", [128, NEXP * 32], st=S); bdb = [C.sb("bdb%d" % i, [128, D], st=S) for i in range(2)]
        gtile = C.sb("gtile", [128, 4, NEXP], st=S)
        hg = C.sb("hg", [128, MOE_BLK], st=S); sg = C.sb("sg", [128, MOE_BLK], st=S); hu = C.sb("hu", [128, MOE_BLK], st=S); tt = C.sb("tt", [128, MOE_BLK], st=S)
        ty = C.sb("ty", [128, 512], st=S)
        psg = [C.ps("psg%d" % i, st=S) for i in range(2)]; psu = [C.ps("psu%d" % i, st=S) for i in range(2)]
        psy = [C.ps("psy%d" % i, st=S) for i in range(4)]
        P.dma("sync", lambda q: q.dma_start(out=bgt[:], in_=bgu), writes=["bgt"])
        it = 0; ib = 0
        for tb in range(NT // MOE_BLK):
            t0 = tb * MOE_BLK
            P.dma("sync", lambda q, t0=t0: q.dma_start(out=xb[:], in_=xT[:, t0:t0 + MOE_BLK].rearrange("(kc p) t -> p kc t", p=128)), reads=["dram_xT"], writes=["xb"])
            P.dma("sync", lambda q, t0=t0: q.dma_start(out=gtile[:], in_=gl[t0:t0 + MOE_BLK, :].rearrange("(s p) e -> p s e", p=128)), reads=["dram_gates"], writes=["gtile"])
            for e in range(NEXP):
                B = bdb[ib % 2]; bk = "bdb%d" % (ib % 2); ib += 1
                P.dma("sync", lambda q, B=B, e=e: q.dma_start(out=B[:], in_=bd[e:e + 1, :].partition_broadcast(128)), writes=[bk])
                P.dma("sync", lambda q, e=e: q.dma_start(out=wdt[:], in_=wd_s[e].rearrange("(fc p) d -> p fc d", p=128)),
                      reads=[wd_s[e].name], writes=["wdt"])
                for fc in range(16):
                    W = wt[it % 2]; wk = "wt%d" % (it % 2); G = psg[it % 2]; U = psu[it % 2]; gk = "psg%d" % (it % 2); uk = "psu%d" % (it % 2)
                    it += 1
                    r0 = fc * 128
                    P.dma("sync", lambda q, W=W, r0=r0, e=e: q.dma_start(out=W[:], in_=wgu_s[e][r0:r0 + 128, :].rearrange("p (kc j) -> p kc j", j=256)),
                          reads=[wgu_s[e].name], writes=[wk])
                    for kc in range(16):
                        P.op("tensor", lambda q, W=W, G=G, kc=kc: q.matmul(G[:], W[:, kc, 0:128], xb[:, kc, :], start=(kc == 0), stop=(kc == 15)),
                             reads=[wk, "xb"], writes=[gk])
                    for kc in range(16):
                        P.op("tensor", lambda q, W=W, U=U, kc=kc: q.matmul(U[:], W[:, kc, 128:256], xb[:, kc, :], start=(kc == 0), stop=(kc == 15)),
                             reads=[wk, "xb"], writes=[uk])
                    c0 = (e * 16 + fc) * 2
                    P.op("vector", lambda q, G=G, c0=c0: q.tensor_scalar(out=hg[:], in0=G[:], scalar1=bgt[:, c0:c0 + 1], scalar2=7.0, op0=ALU.add, op1=ALU.min),
                         reads=[gk, "bgt"], writes=["hg"])
                    P.op("scalar", lambda q: q.activation(out=sg[:], in_=hg[:], func=AF.Sigmoid, scale=1.702), reads=["hg"], writes=["sg"])
                    P.op("vector", lambda q, U=U, c0=c0: q.tensor_scalar(out=hu[:], in0=U[:], scalar1=bgt[:, c0 + 1:c0 + 2], scalar2=7.0, op0=ALU.add, op1=ALU.min),
                         reads=[uk, "bgt"], writes=["hu"])
                    P.op("gpsimd", lambda q: q.tensor_scalar(out=hu[:], in0=hu[:], scalar1=-7.0, scalar2=1.0, op0=ALU.max, op1=ALU.add), reads=["hu"], writes=["hu"])
                    P.op("gpsimd", lambda q: q.tensor_tensor(out=tt[:], in0=hg[:], in1=sg[:], op=ALU.mult), reads=["hg", "sg"], writes=["tt"])
                    P.op("vector", lambda q, fc=fc: q.tensor_tensor(out=act[:, fc, :], in0=tt[:], in1=hu[:], op=ALU.mult), reads=["tt", "hu"], writes=["act"])
                for s in range(4):
                    for cb in range(4):
                        Y = psy[cb]; yk = "psy%d" % cb
                        for fc in range(16):
                            P.op("tensor", lambda q, Y=Y, s=s, cb=cb, fc=fc: q.matmul(Y[:], act[:, fc, s * 128:(s + 1) * 128], wdt[:, fc, cb * 512:(cb + 1) * 512],
                                                                                     start=(fc == 0), stop=(fc == 15)),
                                 reads=["act", "wdt"], writes=[yk])
                        P.op("vector", lambda q, Y=Y, B=B, cb=cb: q.tensor_tensor(out=ty[:], in0=Y[:], in1=B[:, cb * 512:(cb + 1) * 512], op=ALU.add),
                             reads=[yk, bk], writes=["ty"])
                        ya = yacc[:, s, cb * 512:(cb + 1) * 512]
                        if e == 0:
                            P.op("gpsimd", lambda q, ya=ya, s=s, e=e: q.tensor_scalar(out=ya, in0=ty[:], scalar1=gtile[:, s, e:e + 1], scalar2=None, op0=ALU.mult),
                                 reads=["ty", "gtile"], writes=["yacc"])
                        else:
                            P.op("vector", lambda q, ya=ya, s=s, e=e: q.scalar_tensor_tensor(out=ya, in0=ty[:], scalar=gtile[:, s, e:e + 1], in1=ya,
                                                                                             op0=ALU.mult, op1=ALU.add),
                                 reads=["ty", "gtile", "yacc"], writes=["yacc"])
            for s in range(4):
                P.dma("sync", lambda q, s=s, t0=t0: q.dma_start(out=y[t0 + s * 128:t0 + (s + 1) * 128, :], in_=yacc[:, s, :]), reads=["yacc"], writes=["dram_part"])
    P.barrier()


def emit_attn(C, pfx, xT0, w_in, w_out, gains, cosf, sinf, rot, ones, onesb, mix, qT_s):
    P = C.P; C.pfx = pfx
    with contextlib.ExitStack() as S0:
        rot_t = C.sb("rot_t", [128, 128], st=S0); ones_t = C.sb("ones_t", [128, 128], st=S0); onesb_t = C.sb("onesb_t", [128, 128], BF16, st=S0)
        gn = C.sb("gn", [128, 4], st=S0)
        P.dma("sync", lambda q: q.dma_start(out=rot_t[:], in_=rot), writes=["consts"])
        P.dma("sync", lambda q: q.dma_start(out=ones_t[:], in_=ones), writes=["consts"])
        P.dma("sync", lambda q: q.dma_start(out=onesb_t[:], in_=onesb), writes=["consts"])
        P.dma("sync", lambda q: q.dma_start(out=gn[:, 0:2], in_=gains), writes=["consts"])
        P.op("vector", lambda q: q.tensor_scalar(out=gn[:, 2:3], in0=gn[:, 0:1], scalar1=128 ** -0.5, scalar2=None, op0=ALU.mult), reads=["consts"], writes=["consts"])

        KT = C.sb("KT", [128, 4, NT], BF16, st=S0); V = C.sb("V", [128, 32, 512], BF16, st=S0)

        def common(st, tag):
            C.pfx = pfx + tag
            T = rope_temps(C, st); T["rot"] = rot_t; T["ones"] = ones_t
            xb = C.sb("xb", [128, 16, 512], BF16, st=st)
            psq = [C.ps("psq%d" % i, st=st) for i in range(2)]
            cf = C.sb("cf", [128, NT], F32, st=st); sf = C.sb("sf", [128, NT], F32, st=st)
            P.dma("sync", lambda q: q.dma_start(out=cf[:], in_=cosf), writes=["tabf"])
            P.dma("sync", lambda q: q.dma_start(out=sf[:], in_=sinf), writes=["tabf"])
            return T, xb, psq, cf, sf

        with contextlib.ExitStack() as s1b:
            T, xb, psq, cf, sf = common(s1b, "b_")
            wq = C.sb("wq", [128, 16, 2048], BF16, st=s1b)
            qo = [C.sb("qo%d" % i, [128, 512], BF16, st=s1b) for i in range(2)]
            P.dma("gpsimd", lambda q: q.dma_start(out=wq[:], in_=w_in[:, 0:2048].rearrange("(kc p) n -> p kc n", p=128)), writes=["wq"])
            it = 0
            for tb in range(NT // 512):
                t0 = tb * 512
                P.dma("gpsimd", lambda q, t0=t0: q.dma_start(out=xb[:], in_=xT0[:, t0:t0 + 512].rearrange("(kc p) t -> p kc t", p=128)), writes=["xb"])
                for h in range(16):
                    ps = psq[it % 2]; pk = "psq%d" % (it % 2); Q = qo[it % 2]; qk = "qo%d" % (it % 2); it += 1
                    for kc in range(16):
                        P.op("tensor", lambda q, ps=ps, kc=kc, h=h: q.matmul(ps[:], wq[:, kc, h * 128:(h + 1) * 128], xb[:, kc, :], start=(kc == 0), stop=(kc == 15)),
                             reads=["wq", "xb"], writes=[pk])
                    rope_tile(C, T, ps, pk, Q[:], qk, cf[:, t0:t0 + 512], sf[:, t0:t0 + 512], ["tabf"], gn[:, 2:3], True)
                    P.dma("sync", lambda q, Q=Q, h=h, t0=t0: q.dma_start(out=qT_s[h * 128:(h + 1) * 128, t0:t0 + 512], in_=Q[:]), reads=[qk], writes=["qT_s"])
        P.barrier()
        C.pfx = pfx
        with contextlib.ExitStack() as s1a:
            T, xb, psq, cf, sf = common(s1a, "a_")
            wk = C.sb("wk", [128, 16, 512], BF16, st=s1a); wv = C.sb("wv", [128, 16, 512], BF16, st=s1a)
            P.dma("gpsimd", lambda q: q.dma_start(out=wk[:], in_=w_in[:, 2048:2560].rearrange("(kc p) n -> p kc n", p=128)), writes=["wk"])
            P.dma("gpsimd", lambda q: q.dma_start(out=wv[:], in_=w_in[:, 2560:3072].rearrange("(kc p) n -> p kc n", p=128)), writes=["wv"])
            it = 0
            for tb in range(NT // 512):
                t0 = tb * 512
                P.dma("gpsimd", lambda q, t0=t0: q.dma_start(out=xb[:], in_=xT0[:, t0:t0 + 512].rearrange("(kc p) t -> p kc t", p=128)), writes=["xb"])
                for h in range(4):
                    ps = psq[it % 2]; pk = "psq%d" % (it % 2); it += 1
                    for kc in range(16):
                        P.op("tensor", lambda q, ps=ps, kc=kc, h=h: q.matmul(ps[:], wk[:, kc, h * 128:(h + 1) * 128], xb[:, kc, :], start=(kc == 0), stop=(kc == 15)),
                             reads=["wk", "xb"], writes=[pk])
                    rope_tile(C, T, ps, pk, KT[:, h, t0:t0 + 512], "KT", cf[:, t0:t0 + 512], sf[:, t0:t0 + 512], ["tabf"], gn[:, 1:2], True)
                for s in range(4):
                    ps = psq[it % 2]; pk = "psq%d" % (it % 2); it += 1
                    for kc in range(16):
                        P.op("tensor", lambda q, ps=ps, kc=kc, s=s: q.matmul(ps[:], xb[:, kc, s * 128:(s + 1) * 128], wv[:, kc, :], start=(kc == 0), stop=(kc == 15)),
                             reads=["wv", "xb"], writes=[pk])
                    P.op("scalar", lambda q, ps=ps, tb=tb, s=s: q.activation(out=V[:, tb * 4 + s, :], in_=ps[:], func=AF.Copy), reads=[pk], writes=["V"])
        C.pfx = pfx
        P.barrier()
        with contextlib.ExitStack() as s2:
            wo = C.sb("wo", [128, 16, D], BF16, st=s2)
            P.dma("gpsimd", lambda q: q.dma_start(out=wo[:], in_=w_out.rearrange("(kc p) n -> p kc n", p=128)), writes=["wo"])
            OT = C.sb("OT", [128, 16, 512], BF16, st=s2)
            qb = [C.sb("qb%d" % i, [128, 512], BF16, st=s2) for i in range(2)]
            pT = [C.sb("pT%d" % i, [128, 512], BF16, st=s2) for i in range(3)]
            rinv = C.sb("rinv", [128, 512], F32, st=s2); mo = C.sb("mo", [128, 512], F32, st=s2)
            pss = [C.ps("pss%d" % i, st=s2) for i in range(2)]
            pso = [C.ps("pso%d" % i, st=s2) for i in range(2)]; psr = [C.ps("psr%d" % i, st=s2) for i in range(2)]
            psm = [C.ps("psm%d" % i, st=s2) for i in range(2)]
            ih = 0; ik = 0; im = 0
            for tb in range(NT // 512):
                t0 = tb * 512
                for h in range(16):
                    kvh = h // 4
                    Q = qb[ih % 2]; qk = "qb%d" % (ih % 2); O = pso[ih % 2]; ok = "pso%d" % (ih % 2); R = psr[ih % 2]; rk = "psr%d" % (ih % 2); ih += 1
                    P.dma("sync", lambda q, Q=Q, h=h, t0=t0: q.dma_start(out=Q[:], in_=qT_s[h * 128:(h + 1) * 128, t0:t0 + 512]), reads=["qT_s"], writes=[qk])
                    for kt in range(32):
                        Sx = pss[ik % 2]; sk = "pss%d" % (ik % 2); Pt = pT[ik % 3]; pk = "pT%d" % (ik % 3); ik += 1
                        P.op("tensor", lambda q, Sx=Sx, Q=Q, kt=kt, kvh=kvh: q.matmul(Sx[:], KT[:, kvh, kt * 128:(kt + 1) * 128], Q[:], start=True, stop=True),
                             reads=["KT", qk], writes=[sk])
                        P.op("scalar", lambda q, Sx=Sx, Pt=Pt: q.activation(out=Pt[:], in_=Sx[:], func=AF.Exp), reads=[sk], writes=[pk])
                        P.op("tensor", lambda q, O=O, Pt=Pt, kt=kt, kvh=kvh: q.matmul(O[:], V[:, kt, kvh * 128:(kvh + 1) * 128], Pt[:], start=(kt == 0), stop=(kt == 31)),
                             reads=["V", pk], writes=[ok])
                        P.op("tensor", lambda q, R=R, Pt=Pt, kt=kt: q.matmul(R[:], onesb_t[:], Pt[:], start=(kt == 0), stop=(kt == 31)),
                             reads=["consts", pk], writes=[rk])
                    P.op("vector", lambda q, R=R: q.reciprocal(out=rinv[:], in_=R[:]), reads=[rk], writes=["rinv"])
                    P.op("vector", lambda q, O=O, h=h: q.tensor_tensor(out=OT[:, h, :], in0=O[:], in1=rinv[:], op=ALU.mult), reads=[ok, "rinv"], writes=["OT"])
                for s in range(4):
                    for cb in range(4):
                        M = psm[im % 2]; mk = "psm%d" % (im % 2); im += 1
                        for h in range(16):
                            P.op("tensor", lambda q, M=M, h=h, s=s, cb=cb: q.matmul(M[:], OT[:, h, s * 128:(s + 1) * 128], wo[:, h, cb * 512:(cb + 1) * 512],
                                                                                     start=(h == 0), stop=(h == 15)),
                                 reads=["OT", "wo"], writes=[mk])
                        P.op("scalar", lambda q, M=M: q.activation(out=mo[:], in_=M[:], func=AF.Copy), reads=[mk], writes=["mo"])
                        P.dma("sync", lambda q, s=s, cb=cb, t0=t0: q.dma_start(out=mix[t0 + s * 128:t0 + (s + 1) * 128, cb * 512:(cb + 1) * 512], in_=mo[:]),
                              reads=["mo"], writes=["dram_part"])
    P.barrier()


def emit_ret(C, pfx, xTf, wq, wk, wv, wg, cs, sn, dec, w_out, dist, rot, ones, gcol, mix, goT_s):
    P = C.P; C.pfx = pfx
    P.barrier()
    with contextlib.ExitStack() as S0:
        rot_t = C.sb("rot_t", [128, 128], st=S0); ones_t = C.sb("ones_t", [128, 128], st=S0); dist_t = C.sb("dist_t", [128, 512], st=S0)
        gc = C.sb("gc", [128, 2], st=S0)
        lgam = C.sb("lgam", [128, 16], st=S0); nlgam = C.sb("nlgam", [128, 16], st=S0); bcol = C.sb("bcol", [128, 2], st=S0)
        for t, d in [(rot_t, rot), (ones_t, ones), (dist_t, dist), (gc, gcol)]:
            P.dma("sync", lambda q, t=t, d=d: q.dma_start(out=t[:], in_=d), writes=["consts"])
        P.dma("sync", lambda q: q.dma_start(out=lgam[:], in_=dec.partition_broadcast(128)), writes=["lgam"])
        P.op("scalar", lambda q: q.activation(out=lgam[:], in_=lgam[:], func=AF.Exp), reads=["lgam"], writes=["lgam"])
        P.op("scalar", lambda q: q.activation(out=lgam[:], in_=lgam[:], func=AF.Ln, scale=-1.0, bias=1.0), reads=["lgam"], writes=["lgam"])
        P.op("vector", lambda q: q.tensor_scalar(out=nlgam[:], in0=lgam[:], scalar1=-1.0, scalar2=None, op0=ALU.mult), reads=["lgam"], writes=["lgam2"])
        with contextlib.ExitStack() as sh:
            T = {}
            for n in ("qn", "t1", "t2"):
                T[n] = C.sb(n, [128, 512], F32, st=sh)
            T["psa"] = C.ps("psa", st=sh); T["psb"] = C.ps("psb", st=sh); T["rot"] = rot_t; T["ones"] = ones_t
            psa, psb = T["psa"], T["psb"]
            wq_t = C.sb("wq_t", [128, 16, 256], BF16, st=sh); wk_t = C.sb("wk_t", [128, 16, 256], BF16, st=sh)
            wv_t = C.sb("wv_t", [128, 16, 512], BF16, st=sh); wg_t = C.sb("wg_t", [128, 16, 512], BF16, st=sh)
            KT = C.sb("KT", [128, 2, NT], BF16, st=sh); V = C.sb("V", [128, 32, 512], BF16, st=sh)
            QT = C.sb("QT", [128, 2, NT], BF16, st=sh); Gq = C.sb("Gq", [128, 4, 512], BF16, st=sh)
            xb = C.sb("xb", [128, 16, 512], BF16, st=sh); ct = C.sb("ct", [128, 2, 512], F32, st=sh); stb = C.sb("stb", [128, 2, 512], F32, st=sh)
            targ = C.sb("targ", [128, 512], F32, st=sh); e1 = C.sb("e1", [128, 512], F32, st=sh); e2 = C.sb("e2", [128, 512], F32, st=sh)
            ma = C.sb("ma", [128, 512], F32, st=sh); mb = C.sb("mb", [128, 512], F32, st=sh); Dt = C.sb("Dt", [128, 512], F32, st=sh)
            pT = [C.sb("pT%d" % i, [128, 512], BF16, st=sh) for i in range(2)]
            osb = C.sb("osb", [128, 4, 512], F32, st=sh); sqo = C.sb("sqo", [128, 4, 512], F32, st=sh); gtile = C.sb("gtile", [128, 512], BF16, st=sh)
            pss = [C.ps("pss%d" % i, st=sh) for i in range(2)]; pso = [C.ps("pso%d" % i, st=sh) for i in range(4)]
            ip = 0; ik = 0
            for h in range(8):
                lgf = lgam[:, h:h + 1]; lgb = lgam[:, 8 + h:9 + h]; nlgb = nlgam[:, 8 + h:9 + h]
                for (wt_, src, nm) in [(wq_t, wq, "wq_t"), (wk_t, wk, "wk_t"), (wv_t, wv, "wv_t"), (wg_t, wg, "wg_t")]:
                    P.dma("gpsimd", lambda q, wt_=wt_, src=src, h=h: q.dma_start(out=wt_[:], in_=src[h * D:(h + 1) * D, :].rearrange("(kc p) n -> p kc n", p=128)),
                          writes=[nm])
                for tb in range(NT // 512):
                    t0 = tb * 512
                    P.dma("sync", lambda q, t0=t0: q.dma_start(out=xb[:], in_=xTf[:, t0:t0 + 512].rearrange("(kc p) t -> p kc t", p=128)), reads=["dram_xT"], writes=["xb"])
                    P.dma("sync", lambda q, t0=t0: q.dma_start(out=ct[:], in_=cs[:, t0:t0 + 512].rearrange("(dt p) t -> p dt t", p=128)), writes=["tab"])
                    P.dma("sync", lambda q, t0=t0: q.dma_start(out=stb[:], in_=sn[:, t0:t0 + 512].rearrange("(dt p) t -> p dt t", p=128)), writes=["tab"])
                    for (wsrc, wnm, dst, dnm, gi) in [(wk_t, "wk_t", KT, "KT", 1), (wq_t, "wq_t", QT, "QT", 0)]:
                        for dt in range(2):
                            ps = pss[ip % 2]; pk = "pss%d" % (ip % 2); ip += 1
                            for kc in range(16):
                                P.op("tensor", lambda q, ps=ps, kc=kc, dt=dt, wsrc=wsrc: q.matmul(ps[:], wsrc[:, kc, dt * 128:(dt + 1) * 128], xb[:, kc, :],
                                                                                                 start=(kc == 0), stop=(kc == 15)),
                                     reads=[wnm, "xb"], writes=[pk])
                            rope_tile(C, T, ps, pk, dst[:, dt, t0:t0 + 512], dnm, ct[:, dt, :], stb[:, dt, :], ["tab"], gc[:, gi:gi + 1], False)
                    for s in range(4):
                        ps = pss[ip % 2]; pk = "pss%d" % (ip % 2); ip += 1
                        for kc in range(16):
                            P.op("tensor", lambda q, ps=ps, kc=kc, s=s: q.matmul(ps[:], xb[:, kc, s * 128:(s + 1) * 128], wv_t[:, kc, :], start=(kc == 0), stop=(kc == 15)),
                                 reads=["wv_t", "xb"], writes=[pk])
                        P.op("scalar", lambda q, ps=ps, tb=tb, s=s: q.activation(out=V[:, tb * 4 + s, :], in_=ps[:], func=AF.Copy), reads=[pk], writes=["V"])
                for qb in range(NT // 512):
                    ql = qb * 512
                    P.dma("sync", lambda q, ql=ql: q.dma_start(out=xb[:], in_=xTf[:, ql:ql + 512].rearrange("(kc p) t -> p kc t", p=128)), reads=["dram_xT"], writes=["xb"])
                    for dt in range(4):
                        ps = pss[ip % 2]; pk = "pss%d" % (ip % 2); ip += 1
                        for kc in range(16):
                            P.op("tensor", lambda q, ps=ps, kc=kc, dt=dt: q.matmul(ps[:], wg_t[:, kc, dt * 128:(dt + 1) * 128], xb[:, kc, :], start=(kc == 0), stop=(kc == 15)),
                                 reads=["wg_t", "xb"], writes=[pk])
                        P.op("scalar", lambda q, ps=ps, dt=dt: q.activation(out=Gq[:, dt, :], in_=ps[:], func=AF.Silu), reads=[pk], writes=["Gq"])
                    for kt in range(32):
                        k0 = kt * 128; off = float(ql - k0)
                        Sx = pss[ip % 2]; sk = "pss%d" % (ip % 2); ip += 1
                        Pt = pT[ik % 2]; pk = "pT%d" % (ik % 2); ik += 1
                        for dt in range(2):
                            P.op("tensor", lambda q, Sx=Sx, dt=dt, k0=k0, ql=ql: q.matmul(Sx[:], KT[:, dt, k0:k0 + 128], QT[:, dt, ql:ql + 512], start=(dt == 0), stop=(dt == 1)),
                                 reads=["KT", "QT"], writes=[sk])
                        if off >= 128:
                            P.op("vector", lambda q, lgf=lgf, off=off: q.tensor_scalar(out=bcol[:, 0:1], in0=lgf, scalar1=off, scalar2=None, op0=ALU.mult),
                                 reads=["lgam"], writes=["bcol"])
                            P.op("scalar", lambda q, lgf=lgf: q.activation(out=Dt[:], in_=dist_t[:], func=AF.Exp, scale=lgf, bias=bcol[:, 0:1]),
                                 reads=["consts", "lgam", "bcol"], writes=["Dt"])
                        elif off <= -512:
                            P.op("vector", lambda q, nlgb=nlgb, off=off: q.tensor_scalar(out=bcol[:, 0:1], in0=nlgb, scalar1=off, scalar2=None, op0=ALU.mult),
                                 reads=["lgam2"], writes=["bcol"])
                            P.op("scalar", lambda q, nlgb=nlgb: q.activation(out=Dt[:], in_=dist_t[:], func=AF.Exp, scale=nlgb, bias=bcol[:, 0:1]),
                                 reads=["consts", "lgam2", "bcol"], writes=["Dt"])
                        else:
                            P.op("vector", lambda q, off=off: q.tensor_scalar(out=targ[:], in0=dist_t[:], scalar1=off, scalar2=None, op0=ALU.add), reads=["consts"], writes=["targ"])
                            P.op("vector", lambda q: q.tensor_scalar(out=ma[:], in0=targ[:], scalar1=0.0, scalar2=None, op0=ALU.max), reads=["targ"], writes=["ma"])
                            P.op("scalar", lambda q, lgf=lgf: q.activation(out=e1[:], in_=ma[:], func=AF.Exp, scale=lgf), reads=["ma", "lgam"], writes=["e1"])
                            P.op("vector", lambda q: q.tensor_scalar(out=mb[:], in0=targ[:], scalar1=-1.0, scalar2=0.0, op0=ALU.mult, op1=ALU.max), reads=["targ"], writes=["mb"])
                            P.op("scalar", lambda q, lgb=lgb: q.activation(out=e2[:], in_=mb[:], func=AF.Exp, scale=lgb), reads=["mb", "lgam"], writes=["e2"])
                            P.op("vector", lambda q: q.tensor_scalar(out=ma[:], in0=targ[:], scalar1=0.0, scalar2=None, op0=ALU.is_gt), reads=["targ", "e1"], writes=["ma"])
                            P.op("vector", lambda q: q.tensor_tensor(out=e1[:], in0=e1[:], in1=ma[:], op=ALU.mult), reads=["e1", "ma"], writes=["e1"])
                            P.op("vector", lambda q: q.tensor_scalar(out=mb[:], in0=targ[:], scalar1=0.0, scalar2=None, op0=ALU.is_lt), reads=["targ", "e2"], writes=["mb"])
                            P.op("vector", lambda q: q.tensor_tensor(out=e2[:], in0=e2[:], in1=mb[:], op=ALU.mult), reads=["e2", "mb"], writes=["e2"])
                            P.op("vector", lambda q: q.tensor_tensor(out=e1[:], in0=e1[:], in1=e2[:], op=ALU.add), reads=["e1", "e2"], writes=["e1"])
                            P.op("vector", lambda q: q.tensor_scalar(out=ma[:], in0=targ[:], scalar1=0.0, scalar2=None, op0=ALU.is_equal), reads=["targ"], writes=["ma"])
                            P.op("vector", lambda q: q.scalar_tensor_tensor(out=Dt[:], in0=ma[:], scalar=2.0, in1=e1[:], op0=ALU.mult, op1=ALU.add),
                                 reads=["ma", "e1"], writes=["Dt"])
                        P.op("vector", lambda q, Sx=Sx, Pt=Pt: q.tensor_tensor(out=Pt[:], in0=Sx[:], in1=Dt[:], op=ALU.mult), reads=[sk, "Dt"], writes=[pk])
                        for dt in range(4):
                            P.op("tensor", lambda q, dt=dt, kt=kt, Pt=Pt: q.matmul(pso[dt][:], V[:, kt, dt * 128:(dt + 1) * 128], Pt[:], start=(kt == 0), stop=(kt == 31)),
                                 reads=["V", pk], writes=["pso%d" % dt])
                    for dt in range(4):
                        P.op("scalar", lambda q, dt=dt: q.activation(out=osb[:, dt, :], in_=pso[dt][:], func=AF.Copy), reads=["pso%d" % dt], writes=["osb"])
                        P.op("scalar", lambda q, dt=dt: q.activation(out=sqo[:, dt, :], in_=pso[dt][:], func=AF.Square), reads=["pso%d" % dt], writes=["sqo"])
                    for dt in range(4):
                        P.op("tensor", lambda q, dt=dt: q.matmul(psa[:], ones_t[:], osb[:, dt, :], start=(dt == 0), stop=(dt == 3)), reads=["consts", "osb"], writes=["psa"])
                    for dt in range(4):
                        P.op("tensor", lambda q, dt=dt: q.matmul(psb[:], ones_t[:], sqo[:, dt, :], start=(dt == 0), stop=(dt == 3)), reads=["consts", "sqo"], writes=["psb"])
                    P.op("scalar", lambda q: q.activation(out=targ[:], in_=psa[:], func=AF.Copy, scale=1.0 / 512), reads=["psa"], writes=["targ"])
                    P.op("vector", lambda q: q.tensor_tensor(out=e1[:], in0=targ[:], in1=targ[:], op=ALU.mult), reads=["targ"], writes=["e1"])
                    P.op("vector", lambda q: q.scalar_tensor_tensor(out=e2[:], in0=psb[:], scalar=1.0 / 512, in1=e1[:], op0=ALU.mult, op1=ALU.subtract),
                         reads=["psb", "e1"], writes=["e2"])
                    P.op("scalar", lambda q: q.activation(out=e2[:], in_=e2[:], func=AF.Sqrt, bias=1e-5, scale=1.0), reads=["e2"], writes=["e2"])
                    P.op("vector", lambda q: q.reciprocal(out=e2[:], in_=e2[:]), reads=["e2"], writes=["e2"])
                    for dt in range(4):
                        P.op("vector", lambda q, dt=dt: q.tensor_tensor(out=ma[:], in0=osb[:, dt, :], in1=targ[:], op=ALU.subtract), reads=["osb", "targ"], writes=["ma"])
                        P.op("gpsimd", lambda q: q.tensor_tensor(out=mb[:], in0=ma[:], in1=e2[:], op=ALU.mult), reads=["ma", "e2"], writes=["mb"])
                        P.op("vector", lambda q, dt=dt: q.tensor_tensor(out=gtile[:], in0=mb[:], in1=Gq[:, dt, :], op=ALU.mult),
                             reads=["mb", "Gq"], writes=["gtile"])
                        r0 = (h * 4 + dt) * 128
                        P.dma("sync", lambda q, r0=r0, ql=ql: q.dma_start(out=goT_s[r0:r0 + 128, ql:ql + 512], in_=gtile[:]), reads=["gtile"], writes=["goT_s"])
        P.barrier()
        with contextlib.ExitStack() as so:
            wo = C.sb("wo", [128, 32, D], BF16, st=so); gob = C.sb("gob", [128, 32, 512], BF16, st=so); mo = C.sb("mo", [128, 512], F32, st=so)
            psm = [C.ps("psm%d" % i, st=so) for i in range(2)]
            P.dma("gpsimd", lambda q: q.dma_start(out=wo[:], in_=w_out.rearrange("(fc p) n -> p fc n", p=128)), writes=["wo"])
            im = 0
            for qb in range(NT // 512):
                ql = qb * 512
                P.dma("sync", lambda q, ql=ql: q.dma_start(out=gob[:], in_=goT_s[:, ql:ql + 512].rearrange("(fc p) t -> p fc t", p=128)), reads=["goT_s"], writes=["gob"])
                for s in range(4):
                    for cb in range(4):
                        M = psm[im % 2]; mk = "psm%d" % (im % 2); im += 1
                        for fc in range(32):
                            P.op("tensor", lambda q, M=M, fc=fc, s=s, cb=cb: q.matmul(M[:], gob[:, fc, s * 128:(s + 1) * 128], wo[:, fc, cb * 512:(cb + 1) * 512],
                                                                                       start=(fc == 0), stop=(fc == 31)),
                                 reads=["gob", "wo"], writes=[mk])
                        P.op("scalar", lambda q, M=M: q.activation(out=mo[:], in_=M[:], func=AF.Copy), reads=[mk], writes=["mo"])
                        P.dma("sync", lambda q, s=s, cb=cb, ql=ql: q.dma_start(out=mix[ql + s * 128:ql + (s + 1) * 128, cb * 512:(cb + 1) * 512], in_=mo[:]),
                              reads=["mo"], writes=["dram_part"])
    P.barrier()


def build_fused(debug=False):
    C = Ctx(); P = C.P
    x_tm = C.din("x_tm", [NT, D]); xT0 = C.din("xT0", [D, NT])
    a_w_in = C.din("a_w_in", [D, 3072]); a_w_out = C.din("a_w_out", [D, D]); a_gains = C.din("a_gains", [128, 2])
    cos_a = C.din("cos_a", [128, NT]); sin_a = C.din("sin_a", [128, NT])
    rot64 = C.din("rot64", [128, 128]); rot128 = C.din("rot128", [128, 128]); ones = C.din("ones", [128, 128])
    onesb = C.din("onesb", [128, 128], BF16); identb = C.din("identb", [128, 128], BF16)
    lng = [C.din("lng%d" % i, [1, D]) for i in range(4)]; lnb = [C.din("lnb%d" % i, [1, D]) for i in range(4)]
    rwT = [C.din("rwT%d" % i, [32, D]) for i in range(2)]; rb = [C.din("rb%d" % i, [1, 32]) for i in range(2)]
    wgu = [C.din("wgu%d" % i, [NEXP * 16 * 128, 16 * 256]) for i in range(2)]
    wd = [C.din("wd%d" % i, [NEXP * 2048, D]) for i in range(2)]
    bgu = [C.din("bgu%d" % i, [128, NEXP * 32]) for i in range(2)]
    bd = [C.din("bd%d" % i, [NEXP, D]) for i in range(2)]
    r_wq = C.din("r_wq", [8 * D, 256]); r_wk = C.din("r_wk", [8 * D, 256]); r_wv = C.din("r_wv", [8 * D, 512]); r_wg = C.din("r_wg", [8 * D, 512])
    r_cs = C.din("r_cs", [256, NT]); r_sn = C.din("r_sn", [256, NT]); r_dec = C.din("r_dec", [1, 16]); r_w_out = C.din("r_w_out", [4096, D])
    dist = C.din("dist", [128, 512]); gcol = C.din("gcol", [128, 2])
    out = C.dout("out", [NT, D])
    qT_s = C.dscr("qT_s", [D, NT], BF16); goT_s = C.dscr("goT_s", [4096, NT], BF16)
    mk = C.dout if debug else C.dscr
    mixA = mk("mixA", [NT, D]); x1 = mk("x1", [NT, D]); x1T = mk("x1T", [D, NT], BF16); g1 = mk("g1", [NT, 32]); y1 = C.dscr("y1", [NT, D])
    x2 = C.dscr("x2", [NT, D]); x2T = C.dscr("x2T", [D, NT], BF16); mixR = C.dscr("mixR", [NT, D])
    x3 = C.dscr("x3", [NT, D]); x3T = C.dscr("x3T", [D, NT], BF16); g3 = C.dscr("g3", [NT, 32]); y3 = C.dscr("y3", [NT, D])
    wgu_s = [[C.dscr("wgu_s%d_%d" % (i, e), [16 * 128, 16 * 256], BF16) for e in range(NEXP)] for i in range(2)]
    wd_s = [[C.dscr("wd_s%d_%d" % (i, e), [2048, D], BF16) for e in range(NEXP)] for i in range(2)]
    emit_attn(C, "A_", xT0, a_w_in, a_w_out, a_gains, cos_a, sin_a, rot64, ones, onesb, mixA, qT_s)
    if not debug:
        emit_moe_convert(C, wgu[0], wd[0], wgu_s[0], wd_s[0])
        emit_moe_convert(C, wgu[1], wd[1], wgu_s[1], wd_s[1])
    emit_ln(C, "L0_", x_tm, mixA, lng[0], lnb[0], rwT[0], rb[0], identb, x1, x1T, g1)
    if debug:
        return C.finish()
    emit_moe(C, "M0_", x1T, g1, wgu_s[0], wd_s[0], bgu[0], bd[0], y1)
    emit_ln(C, "L1_", x1, y1, lng[1], lnb[1], None, None, identb, x2, x2T, None)
    emit_ret(C, "R_", x2T, r_wq, r_wk, r_wv, r_wg, r_cs, r_sn, r_dec, r_w_out, dist, rot128, ones, gcol, mixR, goT_s)
    emit_ln(C, "L2_", x2, mixR, lng[2], lnb[2], rwT[1], rb[1], identb, x3, x3T, g3)
    emit_moe(C, "M1_", x3T, g3, wgu_s[1], wd_s[1], bgu[1], bd[1], y3)
    emit_ln(C, "L3_", x3, y3, lng[3], lnb[3], None, None, identb, out, None, None)
    return C.finish()


def moe_layout(w_gate_up, b_gate_up, w_down, b_down):
    W = w_gate_up.reshape(NEXP, 16, 128, 16, 128, 2)
    wgu = np.ascontiguousarray(W.transpose(0, 3, 2, 1, 5, 4)).reshape(NEXP * 16 * 128, 16 * 256)
    bg = b_gate_up.reshape(NEXP, 16, 128, 2)
    bgu = np.ascontiguousarray(bg.transpose(2, 0, 1, 3)).reshape(128, NEXP * 32)
    return wgu, np.ascontiguousarray(w_down.reshape(NEXP * 2048, D)), bgu, np.ascontiguousarray(b_down)


_PROG = []


def kernel(x, attn_w_in, attn_q_gain, attn_k_gain, attn_w_out, ret_w_in, ret_decay_fwd, ret_decay_bwd,
           ret_w_out, ln_mix_g, ln_mix_b, router_w, router_b, expert_w_gate_up, expert_b_gate_up,
           expert_w_down, expert_b_down, ln_ffn_g, ln_ffn_b):
    f32 = lambda a: np.asarray(a, dtype=np.float32)
    x = f32(x)
    if not _PROG:
        _PROG.append(build_fused())
    nc = _PROG[0]
    cos_a, sin_a = rope_tables(4096, 128)
    cos_r, sin_r = rope_tables(4096, 256)
    sh = {"a_w_in": np.ascontiguousarray(f32(attn_w_in)[0]), "a_w_out": np.ascontiguousarray(f32(attn_w_out)[0]),
          "a_gains": np.ascontiguousarray(np.stack([f32(attn_q_gain)[0], f32(attn_k_gain)[0]], 1)),
          "cos_a": np.ascontiguousarray(cos_a.T), "sin_a": np.ascontiguousarray(sin_a.T),
          "rot64": rot_lhsT(64), "rot128": rot_lhsT(128), "ones": np.ones((128, 128), np.float32),
          "onesb": np.ones((128, 128), NPBF), "identb": np.eye(128, dtype=np.float32).astype(NPBF)}
    lg = [f32(ln_mix_g)[0], f32(ln_ffn_g)[0], f32(ln_mix_g)[1], f32(ln_ffn_g)[1]]
    lb = [f32(ln_mix_b)[0], f32(ln_ffn_b)[0], f32(ln_mix_b)[1], f32(ln_ffn_b)[1]]
    for i in range(4):
        sh["lng%d" % i] = np.ascontiguousarray(lg[i][None, :]); sh["lnb%d" % i] = np.ascontiguousarray(lb[i][None, :])
    for l in range(2):
        sh["rwT%d" % l] = np.ascontiguousarray(f32(router_w)[l].T); sh["rb%d" % l] = np.ascontiguousarray(f32(router_b)[l][None, :])
        sh["wgu%d" % l], sh["wd%d" % l], sh["bgu%d" % l], sh["bd%d" % l] = moe_layout(
            f32(expert_w_gate_up)[l], f32(expert_b_gate_up)[l], f32(expert_w_down)[l], f32(expert_b_down)[l])
    rw = f32(ret_w_in)[0]
    sh["r_wq"] = np.ascontiguousarray(rw[:, 0:2048].reshape(D, 8, 256).transpose(1, 0, 2)).reshape(8 * D, 256)
    sh["r_wk"] = np.ascontiguousarray(rw[:, 2048:4096].reshape(D, 8, 256).transpose(1, 0, 2)).reshape(8 * D, 256)
    sh["r_wv"] = np.ascontiguousarray(rw[:, 4096:8192].reshape(D, 8, 512).transpose(1, 0, 2)).reshape(8 * D, 512)
    sh["r_wg"] = np.ascontiguousarray(rw[:, 8192:12288].reshape(D, 8, 512).transpose(1, 0, 2)).reshape(8 * D, 512)
    sh["r_cs"] = np.ascontiguousarray(cos_r.T); sh["r_sn"] = np.ascontiguousarray(sin_r.T)
    sh["r_dec"] = np.concatenate([f32(ret_decay_fwd)[0], f32(ret_decay_bwd)[0]]).reshape(1, 16).astype(np.float32)
    sh["r_w_out"] = np.ascontiguousarray(f32(ret_w_out)[0])
    sh["dist"] = (np.arange(512, dtype=np.float32)[None, :] - np.arange(128, dtype=np.float32)[:, None]).astype(np.float32)
    sh["gcol"] = np.stack([np.ones(128, np.float32), np.full(128, 256 ** -0.5, np.float32)], 1)
    maps = []
    for b in range(4):
        m = dict(sh)
        m["x_tm"] = np.ascontiguousarray(x[b]); m["xT0"] = np.ascontiguousarray(x[b].T)
        maps.append(m)
    res = run_bass_kernel_spmd(nc, maps, core_ids=[0, 1, 2, 3])
    return np.stack([res.results[b]["out"] for b in range(4)], 0).astype(np.float32)
```

```python
import contextlib
import math
import numpy as np
import ml_dtypes
import concourse.bass as bass
import concourse.mybir as mybir
from concourse.bass_utils import run_bass_kernel_spmd

F32 = mybir.dt.float32
BF16 = mybir.dt.bfloat16
AF = mybir.ActivationFunctionType
ALU = mybir.AluOpType
AX = mybir.AxisListType
NPBF = ml_dtypes.bfloat16

STREAMS = ("tensor", "vector", "scalar", "gpsimd", "sync")
COUNTERS = ("tensor", "vector", "scalar", "gpsimd", "d_sync", "d_scalar", "d_gpsimd")

D = 2048
ALPHA = 4 ** 0.25
LN_EPS = 1e-5


class Prog:
    def __init__(self, nc):
        self.nc = nc
        self.ops = {s: [] for s in STREAMS}
        self.cnt = {c: 0 for c in COUNTERS}
        self.seen = {s: {c: 0 for c in COUNTERS} for s in STREAMS}
        self.lastw = {}
        self.readers = {}
        self.pending = {s: {} for s in STREAMS}

    def barrier(self):
        for s in STREAMS:
            for c, v in self.cnt.items():
                if c == "d_gpsimd":
                    continue
                if v > self.pending[s].get(c, 0):
                    self.pending[s][c] = v

    def _add(self, stream, counter, inc, fn, reads, writes):
        need = dict(self.pending[stream])
        self.pending[stream] = {}
        for k in reads:
            w = self.lastw.get(k)
            if w is not None:
                need[w[0]] = max(need.get(w[0], 0), w[1])
        for k in writes:
            w = self.lastw.get(k)
            if w is not None:
                need[w[0]] = max(need.get(w[0], 0), w[1])
            for (c, v) in self.readers.get(k, ()):
                need[c] = max(need.get(c, 0), v)
        waits = []
        for c, v in need.items():
            if c == "tensor" and counter == "tensor":
                continue
            if self.seen[stream][c] < v:
                waits.append((c, v))
                self.seen[stream][c] = v
        self.cnt[counter] += inc
        myv = self.cnt[counter]
        self.ops[stream].append((waits, fn, counter, inc))
        for k in reads:
            self.readers.setdefault(k, []).append((counter, myv))
        for k in writes:
            self.lastw[k] = (counter, myv)
            self.readers[k] = []

    def op(self, eng, fn, reads=(), writes=()):
        self._add(eng, eng, 1, fn, reads, writes)

    def dma(self, q, fn, reads=(), writes=()):
        self._add(q, "d_" + q, 16, fn, reads, writes)

    def emit(self):
        nc = self.nc
        with contextlib.ExitStack() as st:
            sems = {c: st.enter_context(nc.semaphore("s_" + c)) for c in COUNTERS}
            block = st.enter_context(nc.Block())
            final_cnt = dict(self.cnt)

            def make(stream):
                def body(eng):
                    for (waits, fn, counter, inc) in self.ops[stream]:
                        for (c, v) in waits:
                            eng.wait_ge(sems[c], v)
                        fn(eng).then_inc(sems[counter], inc)
                    if stream == "sync":
                        for c in COUNTERS:
                            if final_cnt[c] > 0:
                                eng.wait_ge(sems[c], final_cnt[c])
                return body

            block.tensor(make("tensor"))
            block.vector(make("vector"))
            block.scalar(make("scalar"))
            block.gpsimd(make("gpsimd"))
            block.sync(make("sync"))


class Ctx:
    def __init__(self):
        self.nc = bass.Bass("TRN2", target_bir_lowering=False)
        self.P = Prog(self.nc)
        self.st = contextlib.ExitStack()
        self.n = 0

    def din(self, name, shape, dt=F32):
        return self.nc.dram_tensor(name, list(shape), dt, kind="ExternalInput").ap()

    def dout(self, name, shape, dt=F32):
        return self.nc.dram_tensor(name, list(shape), dt, kind="ExternalOutput").ap()

    def dscr(self, name, shape, dt=F32):
        return self.nc.dram_tensor(name, list(shape), dt).ap()

    pfx = ""

    def sb(self, name, shape, dt=F32, st=None):
        return (st or self.st).enter_context(self.nc.sbuf_tensor(self.pfx + name, list(shape), dt))

    def ps(self, name, shape=(128, 512), dt=F32, st=None):
        return (st or self.st).enter_context(self.nc.psum_tensor(self.pfx + name, list(shape), dt))

    def finish(self):
        self.P.emit()
        self.st.close()
        return self.nc


def rope_tile(C, T, src_ps, src_key, out_ap, out_key, cos_ap, sin_ap, tab_keys, gain_ap, rms):
    P = C.P
    q0, sq, rstd, qn, t1, t2 = T.get("q0"), T.get("sq"), T.get("rstd"), T["qn"], T["t1"], T["t2"]
    psa, psb = T["psa"], T["psb"]
    if rms:
        P.op("scalar", lambda q: q.activation(out=q0[:], in_=src_ps[:], func=AF.Copy), reads=[src_key], writes=["q0"])
        P.op("scalar", lambda q: q.activation(out=sq[:], in_=src_ps[:], func=AF.Square), reads=[src_key], writes=["sq"])
        P.op("tensor", lambda q: q.matmul(psa[:], T["ones"][:], sq[:], start=True, stop=True), reads=["sq", "consts"], writes=["psa"])
        P.op("scalar", lambda q: q.activation(out=rstd[:], in_=psa[:], func=AF.Sqrt, scale=1.0 / 128, bias=1e-6), reads=["psa"], writes=["rstd"])
        P.op("vector", lambda q: q.reciprocal(out=rstd[:], in_=rstd[:]), reads=["rstd"], writes=["rstd"])
        P.op("vector", lambda q: q.scalar_tensor_tensor(out=qn[:], in0=q0[:], scalar=gain_ap, in1=rstd[:], op0=ALU.mult, op1=ALU.mult),
             reads=["q0", "rstd", "consts"], writes=["qn"])
    else:
        P.op("scalar", lambda q: q.activation(out=qn[:], in_=src_ps[:], func=AF.Copy, scale=gain_ap), reads=[src_key, "consts"], writes=["qn"])
    P.op("tensor", lambda q: q.matmul(psb[:], T["rot"][:], qn[:], start=True, stop=True), reads=["qn", "consts"], writes=["psb"])
    P.op("gpsimd", lambda q: q.tensor_tensor(out=t1[:], in0=qn[:], in1=cos_ap, op=ALU.mult), reads=["qn"] + tab_keys, writes=["t1"])
    P.op("vector", lambda q: q.tensor_tensor(out=t2[:], in0=psb[:], in1=sin_ap, op=ALU.mult), reads=["psb"] + tab_keys, writes=["t2"])
    P.op("vector", lambda q: q.tensor_tensor(out=out_ap, in0=t1[:], in1=t2[:], op=ALU.add), reads=["t1", "t2"], writes=[out_key])


def rope_temps(C, st):
    T = {}
    for n in ("q0", "sq", "rstd", "qn", "t1", "t2"):
        T[n] = C.sb(n, [128, 512], F32, st=st)
    T["psa"] = C.ps("psa", st=st); T["psb"] = C.ps("psb", st=st)
    return T


def rope_tables(seq, dim):
    rows = seq // 64
    row_idx = np.repeat(np.arange(rows, dtype=np.float32), 64)
    col_idx = np.tile(np.arange(64, dtype=np.float32), rows)
    quarter = dim // 4
    inv_freq = (10000.0 ** (-np.arange(quarter, dtype=np.float32) / quarter)).astype(np.float32)
    ang_r = row_idx[:, None] * inv_freq[None, :]
    ang_c = col_idx[:, None] * inv_freq[None, :]
    ang = np.concatenate([ang_r, ang_r, ang_c, ang_c], axis=-1).astype(np.float32)
    return np.cos(ang).astype(np.float32), np.sin(ang).astype(np.float32)


def rot_lhsT(block):
    R = np.zeros((128, 128), np.float32)
    h = block // 2
    for d in range(128):
        b0 = (d // block) * block; o = d % block
        if o < h:
            R[b0 + o + h, d] = -1.0
        else:
            R[b0 + o - h, d] = 1.0
    return R


NT = 4096
NEXP = 32


def emit_ln(C, pfx, xold, part, g, b, rwT, rb, identb, xnew, xT_out, gates):
    P = C.P; C.pfx = pfx
    P.barrier()
    with contextlib.ExitStack() as S:
        xn = C.sb("xn", [128, 8, D], st=S); pt = C.sb("pt", [128, D], st=S)
        junk = C.sb("junk", [128, D], st=S); jbf = C.sb("jbf", [128, D], BF16, st=S); abf = C.sb("abf", [128, D], BF16, st=S)
        tT = C.sb("tT", [128, 16, 128], BF16, st=S); idb = C.sb("idb", [128, 128], BF16, st=S)
        gb = C.sb("gb", [128, D], st=S); bb = C.sb("bb", [128, D], st=S); rwb = [C.sb("rwb%d" % i, [128, D], st=S) for i in range(2)]
        rbb = C.sb("rbb", [128, 32], st=S); st = C.sb("st", [128, 8], st=S); lg = C.sb("lg", [128, 8, 32], st=S)
        lgt = C.sb("lgt", [128, 32], st=S); m8 = C.sb("m8", [128, 8], st=S); mask = C.sb("mask", [128, 32], st=S); ex = C.sb("ex", [128, 32], st=S)
        sm = C.sb("sm", [128, 4], st=S); gt = C.sb("gt", [128, 32], st=S)
        pst = [C.ps("pst%d" % i, [128, 8, 128], BF16, st=S) for i in range(2)]
        P.dma("sync", lambda q: q.dma_start(out=gb[:], in_=g.partition_broadcast(128)), writes=["gb"])
        P.dma("sync", lambda q: q.dma_start(out=bb[:], in_=b.partition_broadcast(128)), writes=["bb"])
        P.dma("sync", lambda q: q.dma_start(out=idb[:], in_=identb), writes=["idb"])
        if gates is not None:
            P.dma("sync", lambda q: q.dma_start(out=rbb[:], in_=rb.partition_broadcast(128)), writes=["rbb"])
        for grp in range(NT // 1024):
            if gates is not None:
                P.op("vector", lambda q: q.memset(lg[:], 0.0), writes=["lg"])
            for tl in range(8):
                r0 = (grp * 8 + tl) * 128
                A = xn[:, tl, :]; ak = "xn%d" % tl
                P.dma("sync", lambda q, A=A, r0=r0: q.dma_start(out=A, in_=xold[r0:r0 + 128, :]), reads=["dram_x"], writes=[ak])
                P.dma("sync", lambda q, r0=r0: q.dma_start(out=pt[:], in_=part[r0:r0 + 128, :]), reads=["dram_part"], writes=["pt"])
                P.op("vector", lambda q, A=A: q.scalar_tensor_tensor(out=A, in0=A, scalar=ALPHA, in1=pt[:], op0=ALU.mult, op1=ALU.add),
                     reads=[ak, "pt"], writes=[ak])
                P.op("vector", lambda q: q.memset(st[:], 0.0), writes=["st"])
                P.op("scalar", lambda q, A=A: q.activation(out=jbf[:], in_=A, func=AF.Copy, accum_out=st[:, 0:1]), reads=[ak, "st"], writes=["jbf", "st"])
                P.op("scalar", lambda q, A=A: q.activation(out=jbf[:], in_=A, func=AF.Square, accum_out=st[:, 1:2]), reads=[ak, "st"], writes=["jbf", "st"])
                P.op("vector", lambda q: q.tensor_scalar(out=st[:, 2:3], in0=st[:, 0:1], scalar1=1.0 / D, scalar2=None, op0=ALU.mult), reads=["st"], writes=["st"])
                P.op("vector", lambda q: q.tensor_tensor(out=st[:, 3:4], in0=st[:, 2:3], in1=st[:, 2:3], op=ALU.mult), reads=["st"], writes=["st"])
                P.op("vector", lambda q: q.scalar_tensor_tensor(out=st[:, 4:5], in0=st[:, 1:2], scalar=1.0 / D, in1=st[:, 3:4], op0=ALU.mult, op1=ALU.subtract),
                     reads=["st"], writes=["st"])
                P.op("scalar", lambda q: q.activation(out=st[:, 5:6], in_=st[:, 4:5], func=AF.Sqrt, bias=LN_EPS, scale=1.0), reads=["st"], writes=["st"])
                P.op("vector", lambda q: q.reciprocal(out=st[:, 6:7], in_=st[:, 5:6]), reads=["st"], writes=["st"])
                P.op("vector", lambda q, A=A: q.tensor_scalar(out=A, in0=A, scalar1=st[:, 2:3], scalar2=st[:, 6:7], op0=ALU.subtract, op1=ALU.mult),
                     reads=[ak, "st"], writes=[ak])
                P.op("vector", lambda q, A=A: q.tensor_tensor(out=A, in0=A, in1=gb[:], op=ALU.mult), reads=[ak, "gb"], writes=[ak])
                P.op("vector", lambda q, A=A: q.tensor_tensor(out=A, in0=A, in1=bb[:], op=ALU.add), reads=[ak, "bb"], writes=[ak])
                P.dma("sync", lambda q, A=A, r0=r0: q.dma_start(out=xnew[r0:r0 + 128, :], in_=A), reads=[ak], writes=["dram_xnew"])
                if xT_out is not None:
                    P.op("scalar", lambda q, A=A: q.activation(out=abf[:], in_=A, func=AF.Copy), reads=[ak], writes=["abf"])
                    for j in range(16):
                        P.op("tensor", lambda q, j=j: q.transpose(pst[j // 8][:, j % 8, :], abf[:, j * 128:(j + 1) * 128], idb[:]),
                             reads=["abf", "idb"], writes=["pst%d" % (j // 8)])
                    for i2 in range(2):
                        P.op("vector", lambda q, i2=i2: q.tensor_copy(tT[:, i2 * 8:(i2 + 1) * 8, :], pst[i2][:]), reads=["pst%d" % i2], writes=["tT"])
                    P.dma("sync", lambda q, r0=r0: q.dma_start(out=xT_out[:, r0:r0 + 128].rearrange("(kc p) t -> p kc t", p=128), in_=tT[:]),
                          reads=["tT"], writes=["dram_xT"])
            if gates is None:
                continue
            for e in range(32):
                W = rwb[e % 2]; wk = "rwb%d" % (e % 2)
                P.dma("sync", lambda q, W=W, e=e: q.dma_start(out=W[:], in_=rwT[e:e + 1, :].partition_broadcast(128)), writes=[wk])
                for tl in range(8):
                    P.op("vector", lambda q, W=W, tl=tl, e=e: q.scalar_tensor_tensor(out=junk[:], in0=xn[:, tl, :], scalar=1.0, in1=W[:], op0=ALU.mult,
                                                                                      op1=ALU.mult, accum_out=lg[:, tl, e:e + 1]),
                         reads=["xn%d" % tl, wk, "lg"], writes=["junk", "lg"])
            for tl in range(8):
                r0 = (grp * 8 + tl) * 128
                P.op("vector", lambda q, tl=tl: q.tensor_tensor(out=lgt[:], in0=lg[:, tl, :], in1=rbb[:], op=ALU.add), reads=["lg", "rbb"], writes=["lgt"])
                P.op("vector", lambda q: q.max(out=m8[:], in_=lgt[:]), reads=["lgt"], writes=["m8"])
                P.op("vector", lambda q: q.tensor_scalar(out=mask[:], in0=lgt[:], scalar1=m8[:, 3:4], scalar2=None, op0=ALU.is_ge), reads=["lgt", "m8"], writes=["mask"])
                P.op("vector", lambda q: q.tensor_scalar(out=sm[:, 0:1], in0=m8[:, 0:1], scalar1=-1.0, scalar2=None, op0=ALU.mult), reads=["m8"], writes=["sm"])
                P.op("scalar", lambda q: q.activation(out=ex[:], in_=lgt[:], func=AF.Exp, bias=sm[:, 0:1], scale=1.0), reads=["lgt", "sm"], writes=["ex"])
                P.op("vector", lambda q: q.tensor_tensor(out=ex[:], in0=ex[:], in1=mask[:], op=ALU.mult), reads=["ex", "mask"], writes=["ex"])
                P.op("vector", lambda q: q.reduce_sum(out=sm[:, 1:2], in_=ex[:], axis=AX.X), reads=["ex", "sm"], writes=["sm"])
                P.op("vector", lambda q: q.reciprocal(out=sm[:, 2:3], in_=sm[:, 1:2]), reads=["sm"], writes=["sm"])
                P.op("vector", lambda q: q.tensor_scalar(out=gt[:], in0=ex[:], scalar1=sm[:, 2:3], scalar2=None, op0=ALU.mult), reads=["ex", "sm"], writes=["gt"])
                P.dma("sync", lambda q, r0=r0: q.dma_start(out=gates[r0:r0 + 128, :], in_=gt[:]), reads=["gt"], writes=["dram_gates"])
    P.barrier()


MOE_BLK = 512


def emit_moe_convert(C, wgu, wd, wgu_s, wd_s):
    P = C.P
    for e in range(NEXP):
        for j in range(16):
            i = e * 16 + j
            P.dma("gpsimd", lambda q, i=i, e=e, j=j: q.dma_start(out=wgu_s[e][j * 128:(j + 1) * 128, :], in_=wgu[i * 128:(i + 1) * 128, :]), writes=["wconv"])
            P.dma("gpsimd", lambda q, i=i, e=e, j=j: q.dma_start(out=wd_s[e][j * 128:(j + 1) * 128, :], in_=wd[i * 128:(i + 1) * 128, :]), writes=["wconv"])


def emit_moe(C, pfx, xT, gl, wgu_s, wd_s, bgu, bd, y):
    P = C.P; C.pfx = pfx
    P.barrier()
    with contextlib.ExitStack() as S:
        xb = C.sb("xb", [128, 16, MOE_BLK], BF16, st=S); act = C.sb("act", [128, 16, MOE_BLK], BF16, st=S)
        yacc = C.sb("yacc", [128, 4, D], st=S); wdt = C.sb("wdt", [128, 16, D], BF16, st=S)
        wt = [C.sb("wt%d" % i, [128, 16, 256], BF16, st=S) for i in range(3)]
        bgt = C.sb("bgt", [128, NEXP * 32], st=S); bdb = [C.sb("bdb%d" % i, [128, D], st=S) for i in range(2)]
        gtile = C.sb("gtile", [128, 4, NEXP], st=S)
        hg = C.sb("hg", [128, MOE_BLK], st=S); sg = C.sb("sg", [128, MOE_BLK], st=S); hu = C.sb("hu", [128, MOE_BLK], st=S); tt = C.sb("tt", [128, MOE_BLK], st=S)
        ty = C.sb("ty", [128, 512], st=S)
        psg = [C.ps("psg%d" % i, st=S) for i in range(2)]; psu = [C.ps("psu%d" % i, st=S) for i in range(2)]
        psy = [C.ps("psy%d" % i, st=S) for i in range(4)]
        P.dma("sync", lambda q: q.dma_start(out=bgt[:], in_=bgu), writes=["bgt"])
        it = 0; ib = 0
        for tb in range(NT // MOE_BLK):
            t0 = tb * MOE_BLK
            P.dma("sync", lambda q, t0=t0: q.dma_start(out=xb[:], in_=xT[:, t0:t0 + MOE_BLK].rearrange("(kc p) t -> p kc t", p=128)), reads=["dram_xT"], writes=["xb"])
            P.dma("sync", lambda q, t0=t0: q.dma_start(out=gtile[:], in_=gl[t0:t0 + MOE_BLK, :].rearrange("(s p) e -> p s e", p=128)), reads=["dram_gates"], writes=["gtile"])
            for e in range(NEXP):
                B = bdb[ib % 2]; bk = "bdb%d" % (ib % 2); ib += 1
                P.dma("sync", lambda q, B=B, e=e: q.dma_start(out=B[:], in_=bd[e:e + 1, :].partition_broadcast(128)), writes=[bk])
                for fc in range(16):
                    W = wt[it % 3]; wk = "wt%d" % (it % 3); G = psg[it % 2]; U = psu[it % 2]; gk = "psg%d" % (it % 2); uk = "psu%d" % (it % 2)
                    it += 1
                    r0 = fc * 128
                    P.dma("sync", lambda q, W=W, r0=r0, e=e: q.dma_start(out=W[:], in_=wgu_s[e][r0:r0 + 128, :].rearrange("p (kc j) -> p kc j", j=256)),
                          reads=["wconv"], writes=[wk])
                    if fc % 4 == 1:
                        cq = fc // 4
                        P.dma("sync", lambda q, e=e, cq=cq: q.dma_start(out=wdt[:, :, cq * 512:(cq + 1) * 512],
                                                                        in_=wd_s[e][:, cq * 512:(cq + 1) * 512].rearrange("(fc p) d -> p fc d", p=128)),
                              reads=["wconv"], writes=["wdt%d" % cq])
                    for kc in range(16):
                        P.op("tensor", lambda q, W=W, G=G, kc=kc: q.matmul(G[:], W[:, kc, 0:128], xb[:, kc, :], start=(kc == 0), stop=(kc == 15)),
                             reads=[wk, "xb"], writes=[gk])
                    for kc in range(16):
                        P.op("tensor", lambda q, W=W, U=U, kc=kc: q.matmul(U[:], W[:, kc, 128:256], xb[:, kc, :], start=(kc == 0), stop=(kc == 15)),
                             reads=[wk, "xb"], writes=[uk])
                    c0 = (e * 16 + fc) * 2
                    P.op("vector", lambda q, G=G, c0=c0: q.tensor_scalar(out=hg[:], in0=G[:], scalar1=bgt[:, c0:c0 + 1], scalar2=7.0, op0=ALU.add, op1=ALU.min),
                         reads=[gk, "bgt"], writes=["hg"])
                    P.op("scalar", lambda q: q.activation(out=sg[:], in_=hg[:], func=AF.Sigmoid, scale=1.702), reads=["hg"], writes=["sg"])
                    P.op("vector", lambda q, U=U, c0=c0: q.tensor_scalar(out=hu[:], in0=U[:], scalar1=bgt[:, c0 + 1:c0 + 2], scalar2=7.0, op0=ALU.add, op1=ALU.min),
                         reads=[uk, "bgt"], writes=["hu"])
                    P.op("gpsimd", lambda q: q.tensor_scalar(out=hu[:], in0=hu[:], scalar1=-7.0, scalar2=1.0, op0=ALU.max, op1=ALU.add), reads=["hu"], writes=["hu"])
                    P.op("gpsimd", lambda q: q.tensor_tensor(out=tt[:], in0=hg[:], in1=sg[:], op=ALU.mult), reads=["hg", "sg"], writes=["tt"])
                    P.op("vector", lambda q, fc=fc: q.tensor_tensor(out=act[:, fc, :], in0=tt[:], in1=hu[:], op=ALU.mult), reads=["tt", "hu"], writes=["act"])
                for s in range(4):
                    for cb in range(4):
                        Y = psy[cb]; yk = "psy%d" % cb
                        for fc in range(16):
                            P.op("tensor", lambda q, Y=Y, s=s, cb=cb, fc=fc: q.matmul(Y[:], act[:, fc, s * 128:(s + 1) * 128], wdt[:, fc, cb * 512:(cb + 1) * 512],
                                                                                     start=(fc == 0), stop=(fc == 15)),
                                 reads=["act", "wdt%d" % cb], writes=[yk])
                        P.op("vector", lambda q, Y=Y, B=B, cb=cb: q.tensor_tensor(out=ty[:], in0=Y[:], in1=B[:, cb * 512:(cb + 1) * 512], op=ALU.add),
                             reads=[yk, bk], writes=["ty"])
                        ya = yacc[:, s, cb * 512:(cb + 1) * 512]
                        if e == 0:
                            P.op("gpsimd", lambda q, ya=ya, s=s, e=e: q.tensor_scalar(out=ya, in0=ty[:], scalar1=gtile[:, s, e:e + 1], scalar2=None, op0=ALU.mult),
                                 reads=["ty", "gtile"], writes=["yacc"])
                        else:
                            P.op("vector", lambda q, ya=ya, s=s, e=e: q.scalar_tensor_tensor(out=ya, in0=ty[:], scalar=gtile[:, s, e:e + 1], in1=ya,
                                                                                             op0=ALU.mult, op1=ALU.add),
                                 reads=["ty", "gtile", "yacc"], writes=["yacc"])
            for s in range(4):
                P.dma("sync", lambda q, s=s, t0=t0: q.dma_start(out=y[t0 + s * 128:t0 + (s + 1) * 128, :], in_=yacc[:, s, :]), reads=["yacc"], writes=["dram_part"])
    P.barrier()


def emit_attn(C, pfx, xT0, w_in, w_out, gains, cosf, sinf, rot, ones, onesb, mix, qT_s):
    P = C.P; C.pfx = pfx
    with contextlib.ExitStack() as S0:
        rot_t = C.sb("rot_t", [128, 128], st=S0); ones_t = C.sb("ones_t", [128, 128], st=S0); onesb_t = C.sb("onesb_t", [128, 128], BF16, st=S0)
        gn = C.sb("gn", [128, 4], st=S0)
        P.dma("sync", lambda q: q.dma_start(out=rot_t[:], in_=rot), writes=["consts"])
        P.dma("sync", lambda q: q.dma_start(out=ones_t[:], in_=ones), writes=["consts"])
        P.dma("sync", lambda q: q.dma_start(out=onesb_t[:], in_=onesb), writes=["consts"])
        P.dma("sync", lambda q: q.dma_start(out=gn[:, 0:2], in_=gains), writes=["consts"])
        P.op("vector", lambda q: q.tensor_scalar(out=gn[:, 2:3], in0=gn[:, 0:1], scalar1=128 ** -0.5, scalar2=None, op0=ALU.mult), reads=["consts"], writes=["consts"])

        KT = C.sb("KT", [128, 4, NT], BF16, st=S0); V = C.sb("V", [128, 32, 512], BF16, st=S0)

        def common(st, tag):
            C.pfx = pfx + tag
            T = rope_temps(C, st); T["rot"] = rot_t; T["ones"] = ones_t
            xb = C.sb("xb", [128, 16, 512], BF16, st=st)
            psq = [C.ps("psq%d" % i, st=st) for i in range(2)]
            cf = C.sb("cf", [128, NT], F32, st=st); sf = C.sb("sf", [128, NT], F32, st=st)
            P.dma("sync", lambda q: q.dma_start(out=cf[:], in_=cosf), writes=["tabf"])
            P.dma("sync", lambda q: q.dma_start(out=sf[:], in_=sinf), writes=["tabf"])
            return T, xb, psq, cf, sf

        with contextlib.ExitStack() as s1b:
            T, xb, psq, cf, sf = common(s1b, "b_")
            wq = C.sb("wq", [128, 16, 2048], BF16, st=s1b)
            qo = [C.sb("qo%d" % i, [128, 512], BF16, st=s1b) for i in range(2)]
            P.dma("gpsimd", lambda q: q.dma_start(out=wq[:], in_=w_in[:, 0:2048].rearrange("(kc p) n -> p kc n", p=128)), writes=["wq"])
            it = 0
            for tb in range(NT // 512):
                t0 = tb * 512
                P.dma("gpsimd", lambda q, t0=t0: q.dma_start(out=xb[:], in_=xT0[:, t0:t0 + 512].rearrange("(kc p) t -> p kc t", p=128)), writes=["xb"])
                for h in range(16):
                    ps = psq[it % 2]; pk = "psq%d" % (it % 2); Q = qo[it % 2]; qk = "qo%d" % (it % 2); it += 1
                    for kc in range(16):
                        P.op("tensor", lambda q, ps=ps, kc=kc, h=h: q.matmul(ps[:], wq[:, kc, h * 128:(h + 1) * 128], xb[:, kc, :], start=(kc == 0), stop=(kc == 15)),
                             reads=["wq", "xb"], writes=[pk])
                    rope_tile(C, T, ps, pk, Q[:], qk, cf[:, t0:t0 + 512], sf[:, t0:t0 + 512], ["tabf"], gn[:, 2:3], True)
                    P.dma("sync", lambda q, Q=Q, h=h, t0=t0: q.dma_start(out=qT_s[h * 128:(h + 1) * 128, t0:t0 + 512], in_=Q[:]), reads=[qk], writes=["qT_s"])
        P.barrier()
        C.pfx = pfx
        with contextlib.ExitStack() as s1a:
            T, xb, psq, cf, sf = common(s1a, "a_")
            wk = C.sb("wk", [128, 16, 512], BF16, st=s1a); wv = C.sb("wv", [128, 16, 512], BF16, st=s1a)
            P.dma("gpsimd", lambda q: q.dma_start(out=wk[:], in_=w_in[:, 2048:2560].rearrange("(kc p) n -> p kc n", p=128)), writes=["wk"])
            P.dma("gpsimd", lambda q: q.dma_start(out=wv[:], in_=w_in[:, 2560:3072].rearrange("(kc p) n -> p kc n", p=128)), writes=["wv"])
            it = 0
            for tb in range(NT // 512):
                t0 = tb * 512
                P.dma("gpsimd", lambda q, t0=t0: q.dma_start(out=xb[:], in_=xT0[:, t0:t0 + 512].rearrange("(kc p) t -> p kc t", p=128)), writes=["xb"])
                for h in range(4):
                    ps = psq[it % 2]; pk = "psq%d" % (it % 2); it += 1
                    for kc in range(16):
                        P.op("tensor", lambda q, ps=ps, kc=kc, h=h: q.matmul(ps[:], wk[:, kc, h * 128:(h + 1) * 128], xb[:, kc, :], start=(kc == 0), stop=(kc == 15)),
                             reads=["wk", "xb"], writes=[pk])
                    rope_tile(C, T, ps, pk, KT[:, h, t0:t0 + 512], "KT", cf[:, t0:t0 + 512], sf[:, t0:t0 + 512], ["tabf"], gn[:, 1:2], True)
                for s in range(4):
                    ps = psq[it % 2]; pk = "psq%d" % (it % 2); it += 1
                    for kc in range(16):
                        P.op("tensor", lambda q, ps=ps, kc=kc, s=s: q.matmul(ps[:], xb[:, kc, s * 128:(s + 1) * 128], wv[:, kc, :], start=(kc == 0), stop=(kc == 15)),
                             reads=["wv", "xb"], writes=[pk])
                    P.op("scalar", lambda q, ps=ps, tb=tb, s=s: q.activation(out=V[:, tb * 4 + s, :], in_=ps[:], func=AF.Copy), reads=[pk], writes=["V"])
        C.pfx = pfx
        P.barrier()
        with contextlib.ExitStack() as s2:
            wo = C.sb("wo", [128, 16, D], BF16, st=s2)
            P.dma("gpsimd", lambda q: q.dma_start(out=wo[:], in_=w_out.rearrange("(kc p) n -> p kc n", p=128)), writes=["wo"])
            OT = C.sb("OT", [128, 16, 512], BF16, st=s2)
            qb = [C.sb("qb%d" % i, [128, 512], BF16, st=s2) for i in range(2)]
            pT = [C.sb("pT%d" % i, [128, 512], BF16, st=s2) for i in range(3)]
            rinv = C.sb("rinv", [128, 512], F32, st=s2); mo = C.sb("mo", [128, 512], F32, st=s2)
            pss = [C.ps("pss%d" % i, st=s2) for i in range(2)]
            pso = [C.ps("pso%d" % i, st=s2) for i in range(2)]; psr = [C.ps("psr%d" % i, st=s2) for i in range(2)]
            psm = [C.ps("psm%d" % i, st=s2) for i in range(2)]
            ih = 0; ik = 0; im = 0
            for tb in range(NT // 512):
                t0 = tb * 512
                for h in range(16):
                    kvh = h // 4
                    Q = qb[ih % 2]; qk = "qb%d" % (ih % 2); O = pso[ih % 2]; ok = "pso%d" % (ih % 2); R = psr[ih % 2]; rk = "psr%d" % (ih % 2); ih += 1
                    P.dma("sync", lambda q, Q=Q, h=h, t0=t0: q.dma_start(out=Q[:], in_=qT_s[h * 128:(h + 1) * 128, t0:t0 + 512]), reads=["qT_s"], writes=[qk])
                    for kt in range(32):
                        Sx = pss[ik % 2]; sk = "pss%d" % (ik % 2); Pt = pT[ik % 3]; pk = "pT%d" % (ik % 3); ik += 1
                        P.op("tensor", lambda q, Sx=Sx, Q=Q, kt=kt, kvh=kvh: q.matmul(Sx[:], KT[:, kvh, kt * 128:(kt + 1) * 128], Q[:], start=True, stop=True),
                             reads=["KT", qk], writes=[sk])
                        P.op("scalar", lambda q, Sx=Sx, Pt=Pt: q.activation(out=Pt[:], in_=Sx[:], func=AF.Exp), reads=[sk], writes=[pk])
                        P.op("tensor", lambda q, O=O, Pt=Pt, kt=kt, kvh=kvh: q.matmul(O[:], V[:, kt, kvh * 128:(kvh + 1) * 128], Pt[:], start=(kt == 0), stop=(kt == 31)),
                             reads=["V", pk], writes=[ok])
                        P.op("tensor", lambda q, R=R, Pt=Pt, kt=kt: q.matmul(R[:], onesb_t[:], Pt[:], start=(kt == 0), stop=(kt == 31)),
                             reads=["consts", pk], writes=[rk])
                    P.op("vector", lambda q, R=R: q.reciprocal(out=rinv[:], in_=R[:]), reads=[rk], writes=["rinv"])
                    P.op("vector", lambda q, O=O, h=h: q.tensor_tensor(out=OT[:, h, :], in0=O[:], in1=rinv[:], op=ALU.mult), reads=[ok, "rinv"], writes=["OT"])
                for s in range(4):
                    for cb in range(4):
                        M = psm[im % 2]; mk = "psm%d" % (im % 2); im += 1
                        for h in range(16):
                            P.op("tensor", lambda q, M=M, h=h, s=s, cb=cb: q.matmul(M[:], OT[:, h, s * 128:(s + 1) * 128], wo[:, h, cb * 512:(cb + 1) * 512],
                                                                                     start=(h == 0), stop=(h == 15)),
                                 reads=["OT", "wo"], writes=[mk])
                        P.op("scalar", lambda q, M=M: q.activation(out=mo[:], in_=M[:], func=AF.Copy), reads=[mk], writes=["mo"])
                        P.dma("sync", lambda q, s=s, cb=cb, t0=t0: q.dma_start(out=mix[t0 + s * 128:t0 + (s + 1) * 128, cb * 512:(cb + 1) * 512], in_=mo[:]),
                              reads=["mo"], writes=["dram_part"])
    P.barrier()


def emit_ret(C, pfx, xTf, wq, wk, wv, wg, cs, sn, dec, w_out, dist, rot, ones, gcol, mix, goT_s):
    P = C.P; C.pfx = pfx
    P.barrier()
    with contextlib.ExitStack() as S0:
        rot_t = C.sb("rot_t", [128, 128], st=S0); ones_t = C.sb("ones_t", [128, 128], st=S0); dist_t = C.sb("dist_t", [128, 512], st=S0)
        gc = C.sb("gc", [128, 2], st=S0)
        lgam = C.sb("lgam", [128, 16], st=S0); nlgam = C.sb("nlgam", [128, 16], st=S0); bcol = C.sb("bcol", [128, 2], st=S0)
        for t, d in [(rot_t, rot), (ones_t, ones), (dist_t, dist), (gc, gcol)]:
            P.dma("sync", lambda q, t=t, d=d: q.dma_start(out=t[:], in_=d), writes=["consts"])
        P.dma("sync", lambda q: q.dma_start(out=lgam[:], in_=dec.partition_broadcast(128)), writes=["lgam"])
        P.op("scalar", lambda q: q.activation(out=lgam[:], in_=lgam[:], func=AF.Exp), reads=["lgam"], writes=["lgam"])
        P.op("scalar", lambda q: q.activation(out=lgam[:], in_=lgam[:], func=AF.Ln, scale=-1.0, bias=1.0), reads=["lgam"], writes=["lgam"])
        P.op("vector", lambda q: q.tensor_scalar(out=nlgam[:], in0=lgam[:], scalar1=-1.0, scalar2=None, op0=ALU.mult), reads=["lgam"], writes=["lgam2"])
        with contextlib.ExitStack() as sh:
            T = {}
            for n in ("qn", "t1", "t2"):
                T[n] = C.sb(n, [128, 512], F32, st=sh)
            T["psa"] = C.ps("psa", st=sh); T["psb"] = C.ps("psb", st=sh); T["rot"] = rot_t; T["ones"] = ones_t
            psa, psb = T["psa"], T["psb"]
            wq_t = C.sb("wq_t", [128, 16, 256], BF16, st=sh); wk_t = C.sb("wk_t", [128, 16, 256], BF16, st=sh)
            wv_t = C.sb("wv_t", [128, 16, 512], BF16, st=sh); wg_t = C.sb("wg_t", [128, 16, 512], BF16, st=sh)
            KT = C.sb("KT", [128, 2, NT], BF16, st=sh); V = C.sb("V", [128, 32, 512], BF16, st=sh)
            QT = C.sb("QT", [128, 2, NT], BF16, st=sh); Gq = C.sb("Gq", [128, 4, 512], BF16, st=sh)
            xb = C.sb("xb", [128, 16, 512], BF16, st=sh); ct = C.sb("ct", [128, 2, 512], F32, st=sh); stb = C.sb("stb", [128, 2, 512], F32, st=sh)
            targ = C.sb("targ", [128, 512], F32, st=sh); e1 = C.sb("e1", [128, 512], F32, st=sh); e2 = C.sb("e2", [128, 512], F32, st=sh)
            ma = C.sb("ma", [128, 512], F32, st=sh); mb = C.sb("mb", [128, 512], F32, st=sh); Dt = C.sb("Dt", [128, 512], F32, st=sh)
            pT = [C.sb("pT%d" % i, [128, 512], BF16, st=sh) for i in range(2)]
            osb = C.sb("osb", [128, 4, 512], F32, st=sh); sqo = C.sb("sqo", [128, 4, 512], F32, st=sh); gtile = C.sb("gtile", [128, 512], BF16, st=sh)
            pss = [C.ps("pss%d" % i, st=sh) for i in range(2)]; pso = [C.ps("pso%d" % i, st=sh) for i in range(4)]
            ip = 0; ik = 0
            for h in range(8):
                lgf = lgam[:, h:h + 1]; lgb = lgam[:, 8 + h:9 + h]; nlgb = nlgam[:, 8 + h:9 + h]
                for (wt_, src, nm) in [(wq_t, wq, "wq_t"), (wk_t, wk, "wk_t"), (wv_t, wv, "wv_t"), (wg_t, wg, "wg_t")]:
                    P.dma("gpsimd", lambda q, wt_=wt_, src=src, h=h: q.dma_start(out=wt_[:], in_=src[h * D:(h + 1) * D, :].rearrange("(kc p) n -> p kc n", p=128)),
                          writes=[nm])
                for tb in range(NT // 512):
                    t0 = tb * 512
                    P.dma("sync", lambda q, t0=t0: q.dma_start(out=xb[:], in_=xTf[:, t0:t0 + 512].rearrange("(kc p) t -> p kc t", p=128)), reads=["dram_xT"], writes=["xb"])
                    P.dma("sync", lambda q, t0=t0: q.dma_start(out=ct[:], in_=cs[:, t0:t0 + 512].rearrange("(dt p) t -> p dt t", p=128)), writes=["tab"])
                    P.dma("sync", lambda q, t0=t0: q.dma_start(out=stb[:], in_=sn[:, t0:t0 + 512].rearrange("(dt p) t -> p dt t", p=128)), writes=["tab"])
                    for (wsrc, wnm, dst, dnm, gi) in [(wk_t, "wk_t", KT, "KT", 1), (wq_t, "wq_t", QT, "QT", 0)]:
                        for dt in range(2):
                            ps = pss[ip % 2]; pk = "pss%d" % (ip % 2); ip += 1
                            for kc in range(16):
                                P.op("tensor", lambda q, ps=ps, kc=kc, dt=dt, wsrc=wsrc: q.matmul(ps[:], wsrc[:, kc, dt * 128:(dt + 1) * 128], xb[:, kc, :],
                                                                                                 start=(kc == 0), stop=(kc == 15)),
                                     reads=[wnm, "xb"], writes=[pk])
                            rope_tile(C, T, ps, pk, dst[:, dt, t0:t0 + 512], dnm, ct[:, dt, :], stb[:, dt, :], ["tab"], gc[:, gi:gi + 1], False)
                    for s in range(4):
                        ps = pss[ip % 2]; pk = "pss%d" % (ip % 2); ip += 1
                        for kc in range(16):
                            P.op("tensor", lambda q, ps=ps, kc=kc, s=s: q.matmul(ps[:], xb[:, kc, s * 128:(s + 1) * 128], wv_t[:, kc, :], start=(kc == 0), stop=(kc == 15)),
                                 reads=["wv_t", "xb"], writes=[pk])
                        P.op("scalar", lambda q, ps=ps, tb=tb, s=s: q.activation(out=V[:, tb * 4 + s, :], in_=ps[:], func=AF.Copy), reads=[pk], writes=["V"])
                for qb in range(NT // 512):
                    ql = qb * 512
                    P.dma("sync", lambda q, ql=ql: q.dma_start(out=xb[:], in_=xTf[:, ql:ql + 512].rearrange("(kc p) t -> p kc t", p=128)), reads=["dram_xT"], writes=["xb"])
                    for dt in range(4):
                        ps = pss[ip % 2]; pk = "pss%d" % (ip % 2); ip += 1
                        for kc in range(16):
                            P.op("tensor", lambda q, ps=ps, kc=kc, dt=dt: q.matmul(ps[:], wg_t[:, kc, dt * 128:(dt + 1) * 128], xb[:, kc, :], start=(kc == 0), stop=(kc == 15)),
                                 reads=["wg_t", "xb"], writes=[pk])
                        P.op("scalar", lambda q, ps=ps, dt=dt: q.activation(out=Gq[:, dt, :], in_=ps[:], func=AF.Silu), reads=[pk], writes=["Gq"])
                    for kt in range(32):
                        k0 = kt * 128; off = float(ql - k0)
                        Sx = pss[ip % 2]; sk = "pss%d" % (ip % 2); ip += 1
                        Pt = pT[ik % 2]; pk = "pT%d" % (ik % 2); ik += 1
                        for dt in range(2):
                            P.op("tensor", lambda q, Sx=Sx, dt=dt, k0=k0, ql=ql: q.matmul(Sx[:], KT[:, dt, k0:k0 + 128], QT[:, dt, ql:ql + 512], start=(dt == 0), stop=(dt == 1)),
                                 reads=["KT", "QT"], writes=[sk])
                        if off >= 128:
                            P.op("vector", lambda q, lgf=lgf, off=off: q.tensor_scalar(out=bcol[:, 0:1], in0=lgf, scalar1=off, scalar2=None, op0=ALU.mult),
                                 reads=["lgam"], writes=["bcol"])
                            P.op("scalar", lambda q, lgf=lgf: q.activation(out=Dt[:], in_=dist_t[:], func=AF.Exp, scale=lgf, bias=bcol[:, 0:1]),
                                 reads=["consts", "lgam", "bcol"], writes=["Dt"])
                        elif off <= -512:
                            P.op("vector", lambda q, nlgb=nlgb, off=off: q.tensor_scalar(out=bcol[:, 0:1], in0=nlgb, scalar1=off, scalar2=None, op0=ALU.mult),
                                 reads=["lgam2"], writes=["bcol"])
                            P.op("scalar", lambda q, nlgb=nlgb: q.activation(out=Dt[:], in_=dist_t[:], func=AF.Exp, scale=nlgb, bias=bcol[:, 0:1]),
                                 reads=["consts", "lgam2", "bcol"], writes=["Dt"])
                        else:
                            P.op("vector", lambda q, off=off: q.tensor_scalar(out=targ[:], in0=dist_t[:], scalar1=off, scalar2=None, op0=ALU.add), reads=["consts"], writes=["targ"])
                            P.op("vector", lambda q: q.tensor_scalar(out=ma[:], in0=targ[:], scalar1=0.0, scalar2=None, op0=ALU.max), reads=["targ"], writes=["ma"])
                            P.op("scalar", lambda q, lgf=lgf: q.activation(out=e1[:], in_=ma[:], func=AF.Exp, scale=lgf), reads=["ma", "lgam"], writes=["e1"])
                            P.op("vector", lambda q: q.tensor_scalar(out=mb[:], in0=targ[:], scalar1=-1.0, scalar2=0.0, op0=ALU.mult, op1=ALU.max), reads=["targ"], writes=["mb"])
                            P.op("scalar", lambda q, lgb=lgb: q.activation(out=e2[:], in_=mb[:], func=AF.Exp, scale=lgb), reads=["mb", "lgam"], writes=["e2"])
                            P.op("vector", lambda q: q.tensor_scalar(out=ma[:], in0=targ[:], scalar1=0.0, scalar2=None, op0=ALU.is_gt), reads=["targ", "e1"], writes=["ma"])
                            P.op("vector", lambda q: q.tensor_tensor(out=e1[:], in0=e1[:], in1=ma[:], op=ALU.mult), reads=["e1", "ma"], writes=["e1"])
                            P.op("vector", lambda q: q.tensor_scalar(out=mb[:], in0=targ[:], scalar1=0.0, scalar2=None, op0=ALU.is_lt), reads=["targ", "e2"], writes=["mb"])
                            P.op("vector", lambda q: q.tensor_tensor(out=e2[:], in0=e2[:], in1=mb[:], op=ALU.mult), reads=["e2", "mb"], writes=["e2"])
                            P.op("vector", lambda q: q.tensor_tensor(out=e1[:], in0=e1[:], in1=e2[:], op=ALU.add), reads=["e1", "e2"], writes=["e1"])
                            P.op("vector", lambda q: q.tensor_scalar(out=ma[:], in0=targ[:], scalar1=0.0, scalar2=None, op0=ALU.is_equal), reads=["targ"], writes=["ma"])
                            P.op("vector", lambda q: q.scalar_tensor_tensor(out=Dt[:], in0=ma[:], scalar=2.0, in1=e1[:], op0=ALU.mult, op1=ALU.add),
                                 reads=["ma", "e1"], writes=["Dt"])
                        P.op("vector", lambda q, Sx=Sx, Pt=Pt: q.tensor_tensor(out=Pt[:], in0=Sx[:], in1=Dt[:], op=ALU.mult), reads=[sk, "Dt"], writes=[pk])
                        for dt in range(4):
                            P.op("tensor", lambda q, dt=dt, kt=kt, Pt=Pt: q.matmul(pso[dt][:], V[:, kt, dt * 128:(dt + 1) * 128], Pt[:], start=(kt == 0), stop=(kt == 31)),
                                 reads=["V", pk], writes=["pso%d" % dt])
                    for dt in range(4):
                        P.op("scalar", lambda q, dt=dt: q.activation(out=osb[:, dt, :], in_=pso[dt][:], func=AF.Copy), reads=["pso%d" % dt], writes=["osb"])
                        P.op("scalar", lambda q, dt=dt: q.activation(out=sqo[:, dt, :], in_=pso[dt][:], func=AF.Square), reads=["pso%d" % dt], writes=["sqo"])
                    for dt in range(4):
                        P.op("tensor", lambda q, dt=dt: q.matmul(psa[:], ones_t[:], osb[:, dt, :], start=(dt == 0), stop=(dt == 3)), reads=["consts", "osb"], writes=["psa"])
                    for dt in range(4):
                        P.op("tensor", lambda q, dt=dt: q.matmul(psb[:], ones_t[:], sqo[:, dt, :], start=(dt == 0), stop=(dt == 3)), reads=["consts", "sqo"], writes=["psb"])
                    P.op("scalar", lambda q: q.activation(out=targ[:], in_=psa[:], func=AF.Copy, scale=1.0 / 512), reads=["psa"], writes=["targ"])
                    P.op("vector", lambda q: q.tensor_tensor(out=e1[:], in0=targ[:], in1=targ[:], op=ALU.mult), reads=["targ"], writes=["e1"])
                    P.op("vector", lambda q: q.scalar_tensor_tensor(out=e2[:], in0=psb[:], scalar=1.0 / 512, in1=e1[:], op0=ALU.mult, op1=ALU.subtract),
                         reads=["psb", "e1"], writes=["e2"])
                    P.op("scalar", lambda q: q.activation(out=e2[:], in_=e2[:], func=AF.Sqrt, bias=1e-5, scale=1.0), reads=["e2"], writes=["e2"])
                    P.op("vector", lambda q: q.reciprocal(out=e2[:], in_=e2[:]), reads=["e2"], writes=["e2"])
                    for dt in range(4):
                        P.op("vector", lambda q, dt=dt: q.tensor_tensor(out=ma[:], in0=osb[:, dt, :], in1=targ[:], op=ALU.subtract), reads=["osb", "targ"], writes=["ma"])
                        P.op("gpsimd", lambda q: q.tensor_tensor(out=mb[:], in0=ma[:], in1=e2[:], op=ALU.mult), reads=["ma", "e2"], writes=["mb"])
                        P.op("vector", lambda q, dt=dt: q.tensor_tensor(out=gtile[:], in0=mb[:], in1=Gq[:, dt, :], op=ALU.mult),
                             reads=["mb", "Gq"], writes=["gtile"])
                        r0 = (h * 4 + dt) * 128
                        P.dma("sync", lambda q, r0=r0, ql=ql: q.dma_start(out=goT_s[r0:r0 + 128, ql:ql + 512], in_=gtile[:]), reads=["gtile"], writes=["goT_s"])
        P.barrier()
        with contextlib.ExitStack() as so:
            wo = C.sb("wo", [128, 32, D], BF16, st=so); gob = C.sb("gob", [128, 32, 512], BF16, st=so); mo = C.sb("mo", [128, 512], F32, st=so)
            psm = [C.ps("psm%d" % i, st=so) for i in range(2)]
            P.dma("gpsimd", lambda q: q.dma_start(out=wo[:], in_=w_out.rearrange("(fc p) n -> p fc n", p=128)), writes=["wo"])
            im = 0
            for qb in range(NT // 512):
                ql = qb * 512
                P.dma("sync", lambda q, ql=ql: q.dma_start(out=gob[:], in_=goT_s[:, ql:ql + 512].rearrange("(fc p) t -> p fc t", p=128)), reads=["goT_s"], writes=["gob"])
                for s in range(4):
                    for cb in range(4):
                        M = psm[im % 2]; mk = "psm%d" % (im % 2); im += 1
                        for fc in range(32):
                            P.op("tensor", lambda q, M=M, fc=fc, s=s, cb=cb: q.matmul(M[:], gob[:, fc, s * 128:(s + 1) * 128], wo[:, fc, cb * 512:(cb + 1) * 512],
                                                                                       start=(fc == 0), stop=(fc == 31)),
                                 reads=["gob", "wo"], writes=[mk])
                        P.op("scalar", lambda q, M=M: q.activation(out=mo[:], in_=M[:], func=AF.Copy), reads=[mk], writes=["mo"])
                        P.dma("sync", lambda q, s=s, cb=cb, ql=ql: q.dma_start(out=mix[ql + s * 128:ql + (s + 1) * 128, cb * 512:(cb + 1) * 512], in_=mo[:]),
                              reads=["mo"], writes=["dram_part"])
    P.barrier()


def build_fused(debug=False):
    C = Ctx(); P = C.P
    x_tm = C.din("x_tm", [NT, D]); xT0 = C.din("xT0", [D, NT])
    a_w_in = C.din("a_w_in", [D, 3072]); a_w_out = C.din("a_w_out", [D, D]); a_gains = C.din("a_gains", [128, 2])
    cos_a = C.din("cos_a", [128, NT]); sin_a = C.din("sin_a", [128, NT])
    rot64 = C.din("rot64", [128, 128]); rot128 = C.din("rot128", [128, 128]); ones = C.din("ones", [128, 128])
    onesb = C.din("onesb", [128, 128], BF16); identb = C.din("identb", [128, 128], BF16)
    lng = [C.din("lng%d" % i, [1, D]) for i in range(4)]; lnb = [C.din("lnb%d" % i, [1, D]) for i in range(4)]
    rwT = [C.din("rwT%d" % i, [32, D]) for i in range(2)]; rb = [C.din("rb%d" % i, [1, 32]) for i in range(2)]
    wgu = [C.din("wgu%d" % i, [NEXP * 16 * 128, 16 * 256]) for i in range(2)]
    wd = [C.din("wd%d" % i, [NEXP * 2048, D]) for i in range(2)]
    bgu = [C.din("bgu%d" % i, [128, NEXP * 32]) for i in range(2)]
    bd = [C.din("bd%d" % i, [NEXP, D]) for i in range(2)]
    r_wq = C.din("r_wq", [8 * D, 256]); r_wk = C.din("r_wk", [8 * D, 256]); r_wv = C.din("r_wv", [8 * D, 512]); r_wg = C.din("r_wg", [8 * D, 512])
    r_cs = C.din("r_cs", [256, NT]); r_sn = C.din("r_sn", [256, NT]); r_dec = C.din("r_dec", [1, 16]); r_w_out = C.din("r_w_out", [4096, D])
    dist = C.din("dist", [128, 512]); gcol = C.din("gcol", [128, 2])
    out = C.dout("out", [NT, D])
    qT_s = C.dscr("qT_s", [D, NT], BF16); goT_s = C.dscr("goT_s", [4096, NT], BF16)
    mk = C.dout if debug else C.dscr
    mixA = mk("mixA", [NT, D]); x1 = mk("x1", [NT, D]); x1T = mk("x1T", [D, NT], BF16); g1 = mk("g1", [NT, 32]); y1 = C.dscr("y1", [NT, D])
    x2 = C.dscr("x2", [NT, D]); x2T = C.dscr("x2T", [D, NT], BF16); mixR = C.dscr("mixR", [NT, D])
    x3 = C.dscr("x3", [NT, D]); x3T = C.dscr("x3T", [D, NT], BF16); g3 = C.dscr("g3", [NT, 32]); y3 = C.dscr("y3", [NT, D])
    wgu_s = [[C.dscr("wgu_s%d_%d" % (i, e), [16 * 128, 16 * 256], BF16) for e in range(NEXP)] for i in range(2)]
    wd_s = [[C.dscr("wd_s%d_%d" % (i, e), [2048, D], BF16) for e in range(NEXP)] for i in range(2)]
    emit_attn(C, "A_", xT0, a_w_in, a_w_out, a_gains, cos_a, sin_a, rot64, ones, onesb, mixA, qT_s)
    if not debug:
        emit_moe_convert(C, wgu[0], wd[0], wgu_s[0], wd_s[0])
        emit_moe_convert(C, wgu[1], wd[1], wgu_s[1], wd_s[1])
    emit_ln(C, "L0_", x_tm, mixA, lng[0], lnb[0], rwT[0], rb[0], identb, x1, x1T, g1)
    if debug:
        return C.finish()
    emit_moe(C, "M0_", x1T, g1, wgu_s[0], wd_s[0], bgu[0], bd[0], y1)
    emit_ln(C, "L1_", x1, y1, lng[1], lnb[1], None, None, identb, x2, x2T, None)
    emit_ret(C, "R_", x2T, r_wq, r_wk, r_wv, r_wg, r_cs, r_sn, r_dec, r_w_out, dist, rot128, ones, gcol, mixR, goT_s)
    emit_ln(C, "L2_", x2, mixR, lng[2], lnb[2], rwT[1], rb[1], identb, x3, x3T, g3)
    emit_moe(C, "M1_", x3T, g3, wgu_s[1], wd_s[1], bgu[1], bd[1], y3)
    emit_ln(C, "L3_", x3, y3, lng[3], lnb[3], None, None, identb, out, None, None)
    return C.finish()


def moe_layout(w_gate_up, b_gate_up, w_down, b_down):
    W = w_gate_up.reshape(NEXP, 16, 128, 16, 128, 2)
    wgu = np.ascontiguousarray(W.transpose(0, 3, 2, 1, 5, 4)).reshape(NEXP * 16 * 128, 16 * 256)
    bg = b_gate_up.reshape(NEXP, 16, 128, 2)
    bgu = np.ascontiguousarray(bg.transpose(2, 0, 1, 3)).reshape(128, NEXP * 32)
    return wgu, np.ascontiguousarray(w_down.reshape(NEXP * 2048, D)), bgu, np.ascontiguousarray(b_down)


_PROG = []


def kernel(x, attn_w_in, attn_q_gain, attn_k_gain, attn_w_out, ret_w_in, ret_decay_fwd, ret_decay_bwd,
           ret_w_out, ln_mix_g, ln_mix_b, router_w, router_b, expert_w_gate_up, expert_b_gate_up,
           expert_w_down, expert_b_down, ln_ffn_g, ln_ffn_b):
    f32 = lambda a: np.asarray(a, dtype=np.float32)
    x = f32(x)
    if not _PROG:
        _PROG.append(build_fused())
    nc = _PROG[0]
    cos_a, sin_a = rope_tables(4096, 128)
    cos_r, sin_r = rope_tables(4096, 256)
    sh = {"a_w_in": np.ascontiguousarray(f32(attn_w_in)[0]), "a_w_out": np.ascontiguousarray(f32(attn_w_out)[0]),
          "a_gains": np.ascontiguousarray(np.stack([f32(attn_q_gain)[0], f32(attn_k_gain)[0]], 1)),
          "cos_a": np.ascontiguousarray(cos_a.T), "sin_a": np.ascontiguousarray(sin_a.T),
          "rot64": rot_lhsT(64), "rot128": rot_lhsT(128), "ones": np.ones((128, 128), np.float32),
          "onesb": np.ones((128, 128), NPBF), "identb": np.eye(128, dtype=np.float32).astype(NPBF)}
    lg = [f32(ln_mix_g)[0], f32(ln_ffn_g)[0], f32(ln_mix_g)[1], f32(ln_ffn_g)[1]]
    lb = [f32(ln_mix_b)[0], f32(ln_ffn_b)[0], f32(ln_mix_b)[1], f32(ln_ffn_b)[1]]
    for i in range(4):
        sh["lng%d" % i] = np.ascontiguousarray(lg[i][None, :]); sh["lnb%d" % i] = np.ascontiguousarray(lb[i][None, :])
    for l in range(2):
        sh["rwT%d" % l] = np.ascontiguousarray(f32(router_w)[l].T); sh["rb%d" % l] = np.ascontiguousarray(f32(router_b)[l][None, :])
        sh["wgu%d" % l], sh["wd%d" % l], sh["bgu%d" % l], sh["bd%d" % l] = moe_layout(
            f32(expert_w_gate_up)[l], f32(expert_b_gate_up)[l], f32(expert_w_down)[l], f32(expert_b_down)[l])
    rw = f32(ret_w_in)[0]
    sh["r_wq"] = np.ascontiguousarray(rw[:, 0:2048].reshape(D, 8, 256).transpose(1, 0, 2)).reshape(8 * D, 256)
    sh["r_wk"] = np.ascontiguousarray(rw[:, 2048:4096].reshape(D, 8, 256).transpose(1, 0, 2)).reshape(8 * D, 256)
    sh["r_wv"] = np.ascontiguousarray(rw[:, 4096:8192].reshape(D, 8, 512).transpose(1, 0, 2)).reshape(8 * D, 512)
    sh["r_wg"] = np.ascontiguousarray(rw[:, 8192:12288].reshape(D, 8, 512).transpose(1, 0, 2)).reshape(8 * D, 512)
    sh["r_cs"] = np.ascontiguousarray(cos_r.T); sh["r_sn"] = np.ascontiguousarray(sin_r.T)
    sh["r_dec"] = np.concatenate([f32(ret_decay_fwd)[0], f32(ret_decay_bwd)[0]]).reshape(1, 16).astype(np.float32)
    sh["r_w_out"] = np.ascontiguousarray(f32(ret_w_out)[0])
    sh["dist"] = (np.arange(512, dtype=np.float32)[None, :] - np.arange(128, dtype=np.float32)[:, None]).astype(np.float32)
    sh["gcol"] = np.stack([np.ones(128, np.float32), np.full(128, 256 ** -0.5, np.float32)], 1)
    maps = []
    for b in range(4):
        m = dict(sh)
        m["x_tm"] = np.ascontiguousarray(x[b]); m["xT0"] = np.ascontiguousarray(x[b].T)
        maps.append(m)
    res = run_bass_kernel_spmd(nc, maps, core_ids=[0, 1, 2, 3])
    return np.stack([res.results[b]["out"] for b in range(4)], 0).astype(np.float32)
```
